# Optimizing a Trainium2 kernel written in Bass

```python
import jax, jax.numpy as jnp
from jax import lax
import numpy as np

D_MODEL = 1024
BATCH = 8
SEQ = 4096
DEPTH = 1

EPS = 1e-6
CHUNK = 64
GLA_HEADS = 4
GLA_DK = 128
GLA_DV = 256
GLA_QK = GLA_HEADS * GLA_DK
GLA_V = GLA_HEADS * GLA_DV
GLA_GATE_RANK = 16
GLA_GATE_TEMP = 16.0
RET_HEADS = 4
RET_DK = 256
RET_DV = 512
RET_QK = RET_HEADS * RET_DK
RET_V = RET_HEADS * RET_DV
ROPE_BASE = 10000.0
IN_SPLITS = (GLA_QK, GLA_QK, GLA_V, GLA_V, GLA_GATE_RANK, RET_QK, RET_QK, RET_V, RET_V, D_MODEL, D_MODEL)
D_IN = sum(IN_SPLITS)
N_GROUPS = 4
EXPERTS_PER_GROUP = 8
N_EXPERTS = N_GROUPS * EXPERTS_PER_GROUP
TOP_K_IN_GROUP = 2
EXPERT_HIDDEN = 512
EXPERT_BLOCK = 128

kernel_name = "hybrid_gla_retnet_hmoe_block"


def _rmsnorm(x, w):
    x32 = x.astype(jnp.float32)
    y = x32 * lax.rsqrt(jnp.mean(x32 * x32, axis=-1, keepdims=True) + EPS)
    return (y * w.astype(jnp.float32)).astype(x.dtype)


def _group_norm(o, w):
    mu = jnp.mean(o, axis=-1, keepdims=True)
    var = jnp.mean(jnp.square(o - mu), axis=-1, keepdims=True)
    y = ((o - mu) * lax.rsqrt(var + EPS)).reshape(o.shape[0], o.shape[1], -1)
    return y * w.astype(jnp.float32)


def _to_chunks(t):
    b, h, s, d = t.shape
    return t.reshape(b, h, s // CHUNK, CHUNK, d).transpose(2, 0, 1, 3, 4)


def _from_chunks(t):
    n, b, h, c, d = t.shape
    return t.transpose(1, 0, 3, 2, 4).reshape(b, n * c, h, d)


def _gla_chunked(q, k, v, log_a):
    b, h, _, dk = q.shape
    dv = v.shape[-1]
    causal = jnp.tril(jnp.ones((CHUNK, CHUNK), dtype=bool))[:, :, None]

    def step(state, inp):
        qi, ki, vi, ai = inp
        cum = jnp.cumsum(ai, axis=-2)
        diff = cum[..., :, None, :] - cum[..., None, :, :]
        decay = jnp.exp(jnp.where(causal, diff, -jnp.inf))
        scores = jnp.einsum("bhid,bhijd,bhjd->bhij", qi, decay, ki)
        o = (jnp.einsum("bhij,bhjv->bhiv", scores, vi)
             + jnp.einsum("bhid,bhdv->bhiv", qi * jnp.exp(cum), state))
        last = cum[..., -1:, :]
        state = (jnp.exp(last[..., 0, :])[..., None] * state
                 + jnp.einsum("bhjd,bhjv->bhdv", ki * jnp.exp(last - cum), vi))
        return state, o

    init = jnp.zeros((b, h, dk, dv), jnp.float32)
    xs = tuple(_to_chunks(t.astype(jnp.float32)) for t in (q, k, v, log_a))
    _, out = lax.scan(step, init, xs)
    return _from_chunks(out)


def _retention_chunked(q, k, v):
    b, h, _, dk = q.shape
    dv = v.shape[-1]
    log_gamma = jnp.log(1.0 - 2.0 ** (-5.0 - jnp.arange(h, dtype=jnp.float32)))
    idx = jnp.arange(CHUNK, dtype=jnp.float32)
    rel = idx[:, None] - idx[None, :]
    intra = jnp.where(rel >= 0, jnp.exp(log_gamma[:, None, None] * jnp.maximum(rel, 0.0)), 0.0)
    q_dec = jnp.exp(log_gamma[:, None] * (idx + 1.0))[:, :, None]
    k_dec = jnp.exp(log_gamma[:, None] * (CHUNK - 1.0 - idx))[:, :, None]
    chunk_dec = jnp.exp(log_gamma * CHUNK)[:, None, None]

    def step(state, inp):
        qi, ki, vi = inp
        scores = jnp.einsum("bhid,bhjd->bhij", qi, ki) * intra
        o = (jnp.einsum("bhij,bhjv->bhiv", scores, vi)
             + jnp.einsum("bhid,bhdv->bhiv", qi * q_dec, state))
        state = chunk_dec * state + jnp.einsum("bhjd,bhjv->bhdv", ki * k_dec, vi)
        return state, o

    init = jnp.zeros((b, h, dk, dv), jnp.float32)
    xs = tuple(_to_chunks(t.astype(jnp.float32)) for t in (q, k, v))
    _, out = lax.scan(step, init, xs)
    return _from_chunks(out)


def _rotary(t, positions):
    dk = t.shape[-1]
    theta = 1.0 / (ROPE_BASE ** jnp.linspace(0.0, 1.0, dk // 2, dtype=jnp.float32))
    theta = jnp.repeat(theta, 2)
    ang = positions.astype(jnp.float32)[:, :, None, None] * theta
    t32 = t.astype(jnp.float32)
    rot = jnp.stack([-t32[..., 1::2], t32[..., 0::2]], axis=-1).reshape(t.shape)
    return t32 * jnp.cos(ang) + rot * jnp.sin(ang)


def _hybrid_mixer(u, positions, w_in, gk_up, gk_bias, gla_norm_w, w_br_gla, ret_norm_w, w_br_ret, w_out):
    B, S, _ = u.shape
    proj = u @ w_in
    splits = np.cumsum(IN_SPLITS)[:-1].tolist()
    gq, gk, gv, gg, gdown, rq, rk, rv, rg, mga, mgb = jnp.split(proj, splits, axis=-1)

    def heads(t, h):
        return t.reshape(B, S, h, -1).transpose(0, 2, 1, 3)

    log_a = jax.nn.log_sigmoid((gdown @ gk_up + gk_bias).astype(jnp.float32)) / GLA_GATE_TEMP
    o_gla = _gla_chunked(heads(gq * (GLA_DK ** -0.5), GLA_HEADS), heads(gk, GLA_HEADS),
                         heads(gv, GLA_HEADS), heads(log_a, GLA_HEADS))
    o_gla = _rmsnorm(o_gla, gla_norm_w).reshape(B, S, GLA_V) * jax.nn.silu(gg.astype(jnp.float32))

    q_r = _rotary(rq.reshape(B, S, RET_HEADS, RET_DK), positions).transpose(0, 2, 1, 3)
    k_r = (_rotary(rk.reshape(B, S, RET_HEADS, RET_DK), positions) * (RET_DK ** -0.5)).transpose(0, 2, 1, 3)
    o_ret = _retention_chunked(q_r, k_r, heads(rv, RET_HEADS))
    o_ret = _group_norm(o_ret, ret_norm_w) * jax.nn.silu(rg.astype(jnp.float32))

    y_a = o_gla.astype(u.dtype) @ w_br_gla
    y_b = o_ret.astype(u.dtype) @ w_br_ret
    merged = (jax.nn.sigmoid(mga.astype(jnp.float32)) * y_a.astype(jnp.float32)
              + jax.nn.sigmoid(mgb.astype(jnp.float32)) * y_b.astype(jnp.float32))
    return merged.astype(u.dtype) @ w_out


def _hierarchical_moe(u, rg_w, rg_b, re_w, re_b, wg, wu, wd):
    B, S, D = u.shape
    t = u.reshape(-1, D)
    n = t.shape[0]
    g_logits = (t @ rg_w + rg_b).astype(jnp.float32)
    g_prob = jax.nn.softmax(g_logits, axis=-1)
    g_idx = jnp.argmax(g_logits, axis=-1)
    g_w = jnp.take_along_axis(g_prob, g_idx[:, None], axis=-1)
    e_all = (jnp.einsum("nd,gde->nge", t, re_w) + re_b).astype(jnp.float32)
    e_logits = jnp.take_along_axis(e_all, g_idx[:, None, None], axis=1)[:, 0]
    top_v, top_i = lax.top_k(e_logits, TOP_K_IN_GROUP)
    weights = g_w * jax.nn.softmax(top_v, axis=-1)
    expert_ids = g_idx[:, None] * EXPERTS_PER_GROUP + top_i

    n_assign = n * TOP_K_IN_GROUP
    flat_e = expert_ids.reshape(-1)
    flat_w = weights.reshape(-1)
    flat_tok = jnp.repeat(jnp.arange(n, dtype=jnp.int32), TOP_K_IN_GROUP)
    order = jnp.argsort(flat_e)
    sorted_e = flat_e[order]
    counts = jnp.bincount(flat_e, length=N_EXPERTS)
    padded = ((counts + EXPERT_BLOCK - 1) // EXPERT_BLOCK) * EXPERT_BLOCK
    pad_end = jnp.cumsum(padded)
    pad_start = pad_end - padded
    start = jnp.cumsum(counts) - counts
    dest = pad_start[sorted_e] + (jnp.arange(n_assign) - start[sorted_e])
    n_slots = ((n_assign + N_EXPERTS * (EXPERT_BLOCK - 1) + EXPERT_BLOCK - 1) // EXPERT_BLOCK) * EXPERT_BLOCK
    n_blocks = n_slots // EXPERT_BLOCK
    slot_tok = jnp.zeros((n_slots,), jnp.int32).at[dest].set(flat_tok[order])
    slot_w = jnp.zeros((n_slots,), jnp.float32).at[dest].set(flat_w[order])
    block_start = jnp.arange(n_blocks) * EXPERT_BLOCK
    block_expert = jnp.minimum(jnp.searchsorted(pad_end, block_start, side="right"), N_EXPERTS - 1)

    def run_block(args):
        e, tok, w = args
        xb = t[tok]
        hid = jax.nn.silu(xb @ wg[e]) * (xb @ wu[e])
        return ((hid @ wd[e]).astype(jnp.float32) * w[:, None])

    outs = lax.map(run_block, (block_expert, slot_tok.reshape(n_blocks, EXPERT_BLOCK),
                               slot_w.reshape(n_blocks, EXPERT_BLOCK)))
    y = jnp.zeros((n, D), jnp.float32).at[slot_tok].add(outs.reshape(n_slots, D))
    return y.reshape(B, S, D).astype(u.dtype)


def setup_inputs(seed: int = 0) -> dict:
    key = jax.random.key(seed)
    ks = jax.random.split(key, 24)
    f32 = jnp.float32

    def nrm(k, shape, scale):
        return jax.random.normal(k, shape, f32) * scale

    x = jax.random.normal(ks[0], (BATCH, SEQ, D_MODEL), f32)
    offsets = jax.random.randint(ks[1], (BATCH, 1), 0, 512)
    positions = (jnp.arange(SEQ, dtype=jnp.int32)[None, :] + offsets).astype(jnp.int32)
    return {
        "x": x,
        "positions": positions,
        "norm_mix_w": 1.0 + nrm(ks[2], (DEPTH, D_MODEL), 0.02),
        "w_in": nrm(ks[3], (DEPTH, D_MODEL, D_IN), D_MODEL ** -0.5),
        "gla_gk_up": nrm(ks[4], (DEPTH, GLA_GATE_RANK, GLA_QK), GLA_GATE_RANK ** -0.5),
        "gla_gk_bias": nrm(ks[5], (DEPTH, GLA_QK), 0.1),
        "gla_norm_w": 1.0 + nrm(ks[6], (DEPTH, GLA_DV), 0.02),
        "w_branch_gla": nrm(ks[7], (DEPTH, GLA_V, D_MODEL), GLA_V ** -0.5),
        "ret_norm_w": 1.0 + nrm(ks[8], (DEPTH, RET_V), 0.02),
        "w_branch_ret": nrm(ks[9], (DEPTH, RET_V, D_MODEL), RET_V ** -0.5),
        "w_out": nrm(ks[10], (DEPTH, D_MODEL, D_MODEL), D_MODEL ** -0.5),
        "norm_ffn_w": 1.0 + nrm(ks[11], (DEPTH, D_MODEL), 0.02),
        "router_group_w": nrm(ks[12], (DEPTH, D_MODEL, N_GROUPS), D_MODEL ** -0.5),
        "router_group_b": nrm(ks[13], (DEPTH, N_GROUPS), 0.01),
        "router_expert_w": nrm(ks[14], (DEPTH, N_GROUPS, D_MODEL, EXPERTS_PER_GROUP), D_MODEL ** -0.5),
        "router_expert_b": nrm(ks[15], (DEPTH, N_GROUPS, EXPERTS_PER_GROUP), 0.01),
        "expert_w_gate": nrm(ks[16], (DEPTH, N_EXPERTS, D_MODEL, EXPERT_HIDDEN), D_MODEL ** -0.5),
        "expert_w_up": nrm(ks[17], (DEPTH, N_EXPERTS, D_MODEL, EXPERT_HIDDEN), D_MODEL ** -0.5),
        "expert_w_down": nrm(ks[18], (DEPTH, N_EXPERTS, EXPERT_HIDDEN, D_MODEL), EXPERT_HIDDEN ** -0.5),
        "norm_final_w": 1.0 + nrm(ks[19], (D_MODEL,), 0.02),
    }


def reference(x, positions, norm_mix_w, w_in, gla_gk_up, gla_gk_bias, gla_norm_w, w_branch_gla,
              ret_norm_w, w_branch_ret, w_out, norm_ffn_w, router_group_w, router_group_b,
              router_expert_w, router_expert_b, expert_w_gate, expert_w_up, expert_w_down, norm_final_w):
    h = x
    for l in range(DEPTH):
        mix = _hybrid_mixer(_rmsnorm(h, norm_mix_w[l]), positions, w_in[l], gla_gk_up[l], gla_gk_bias[l],
                            gla_norm_w[l], w_branch_gla[l], ret_norm_w[l], w_branch_ret[l], w_out[l])
        h = h + mix.astype(h.dtype)
        ffn = _hierarchical_moe(_rmsnorm(h, norm_ffn_w[l]), router_group_w[l], router_group_b[l],
                                router_expert_w[l], router_expert_b[l], expert_w_gate[l],
                                expert_w_up[l], expert_w_down[l])
        h = h + ffn.astype(h.dtype)
    return _rmsnorm(h, norm_final_w)
```

```python
import os
import math
from contextlib import ExitStack
import numpy as np
import concourse.bass as bass
import concourse.mybir as mybir
from concourse.bass_utils import run_bass_kernel_spmd

F32 = mybir.dt.float32
F32R = mybir.dt.float32r
I32 = mybir.dt.int32
AF = mybir.ActivationFunctionType
ALU = mybir.AluOpType
AX = mybir.AxisListType

NCORES = 8
KVAR = int(os.environ.get('KVAR', '0'))
KSUB = int(os.environ.get('KSUB', '9'))
SEQ = 4096
D = 1024
EPS = 1e-6
NQ = 4
QT = 1024
NCOLS_N = 17280
NCOLS_R = 31872

O_GQ, O_GK, O_GV, O_GG, O_GD = 0, 512, 1024, 2048, 3072
O_RQ, O_RK, O_RV, O_RG = 3088, 4112, 5136, 7184
O_MGA, O_MGB = 9232, 10256
D_IN = 11280

F_GQ = [2 * h for h in range(4)]
F_GK = [2 * h + 1 for h in range(4)]
F_RQE = [8 + 4 * h for h in range(4)]
F_RQO = [9 + 4 * h for h in range(4)]
F_RKE = [10 + 4 * h for h in range(4)]
F_RKO = [11 + 4 * h for h in range(4)]
F_MGA = [24 + i for i in range(8)]
F_MGB = [32 + i for i in range(8)]
NF = 40
T_GLA = [h for h in range(4)]
T_RV = [4 + h for h in range(4)]
T_RG = [8 + h for h in range(4)]
NT = 12

C_ID, C_GM, C_RM, C_RV, C_TH, C_NMW, C_NFW, C_GKB = 0, 128, 256, 768, 784, 800, 808, 816
C_LS, C_BS, C_PI = 832, 960, 1024
NCST = 1088
BS = 384
ST = BS // 128
NB = (8192 + 32 * (BS - 1) + BS - 1) // BS
NSLOT = NB * BS
B_GNW, B_RNW, B_NF, B_RB, B_NFW = 0, 256, 2304, 3328, 3392
NBC = 4416


def _esz(dt):
    return 4


class Prog:
    ENGS = ("pe", "act", "dve", "pool", "sp")
    NDMA = 8

    def __init__(self):
        self.ops = {e: [] for e in self.ENGS}
        self.cnt = {e: 0 for e in self.ENGS}
        self.seen = {e: {} for e in self.ENGS}
        self.lastw = {}
        self.lastw_plain = {}
        self.readers = {}
        self.dma_uses = {}
        self.dma_next = {e: 0 for e in self.ENGS}
        self.final = {}
        self.label = ""
        self.labels = {e: [] for e in self.ENGS}

    @staticmethod
    def atoms(ap):
        space = str(ap.space)
        name = ap.tensor.name
        pairs = [(int(s), int(c)) for s, c in ap.ap]
        off = int(ap.offset)
        if space in ("SB", "PSUM"):
            rowstep = pairs[0][0]
            col0 = off % rowstep if rowstep > 0 else off
            ext = sum((c - 1) * abs(s) for s, c in pairs[1:])
            g = 64 if space == "SB" else 512
            return [(name, i) for i in range(col0 // g, (col0 + ext) // g + 1)]
        ext = sum((c - 1) * abs(s) for s, c in pairs)
        g = 16384
        return [(name, i) for i in range(off // g, (off + ext) // g + 1)]

    def add(self, eng, fn, reads, writes, dma=False, merge=False, after=()):
        deps = {}

        def need(ev):
            if ev is None:
                return
            for k, v in ev.items():
                if deps.get(k, 0) < v:
                    deps[k] = v

        writes = list(writes) + [ap for ap in reads if str(ap.space) == "PSUM"]
        reads = [ap for ap in reads if str(ap.space) != "PSUM"]
        for ap in after:
            for a in self.atoms(ap):
                need(self.lastw_plain.get(a))
        ratoms = []
        for ap in reads:
            for a in self.atoms(ap):
                ratoms.append(a)
                need(self.lastw.get(a))
        watoms = []
        for ap in writes:
            for a in self.atoms(ap):
                watoms.append(a)
                if not merge:
                    need(self.lastw.get(a))
                need(self.readers.get(a))
        if dma:
            k = self.dma_next[eng]
            self.dma_next[eng] = (k + 1) % self.NDMA
            key = ("dma", eng, k)
            u = self.dma_uses.get(key, 0)
            if u > 0:
                need({key: 16 * u})
            self.dma_uses[key] = u + 1
            ev = (key, 16 * (u + 1))
        else:
            self.cnt[eng] += 1
            ev = (eng, self.cnt[eng])
        self.final[ev[0]] = ev[1]
        waits = []
        seen = self.seen[eng]
        for k, v in deps.items():
            if k == eng and eng == "pe":
                continue
            if seen.get(k, 0) >= v:
                continue
            seen[k] = v
            waits.append((k, v))
        self.ops[eng].append((waits, fn, ev, dma))
        self.labels[eng].append(self.label)
        for a in ratoms:
            r = self.readers.setdefault(a, {})
            if r.get(ev[0], 0) < ev[1]:
                r[ev[0]] = ev[1]
        for a in watoms:
            if merge:
                lw = self.lastw.setdefault(a, {})
                if lw.get(ev[0], 0) < ev[1]:
                    lw[ev[0]] = ev[1]
            else:
                self.lastw[a] = {ev[0]: ev[1]}
                self.lastw_plain[a] = {ev[0]: ev[1]}
            self.readers[a] = {}

    def emit(self, nc, stack):
        keys = list(self.final.keys())
        sems = {}
        for i, k in enumerate(keys):
            sems[k] = stack.enter_context(nc.semaphore("s%d" % i))
        block = stack.enter_context(nc.Block())

        def mk(name):
            def body(eng):
                for waits, fn, ev, dma in self.ops[name]:
                    for k, v in waits:
                        eng.wait_ge(sems[k], v)
                    ins = fn(eng)
                    ins.then_inc(sems[ev[0]], 16 if dma else 1)
                if name == "sp":
                    for k, v in self.final.items():
                        eng.wait_ge(sems[k], v)
            return body

        block.tensor(mk("pe"))
        block.scalar(mk("act"))
        block.vector(mk("dve"))
        block.gpsimd(mk("pool"))
        block.sync(mk("sp"))


def build(stage=99, debug=False):
    nc = bass.Bass("TRN2", target_bir_lowering=False)
    nc.dge_precook = False
    P = Prog()

    def din(name, shape, dt=F32):
        return nc.dram_tensor(name, list(shape), dt, kind="ExternalInput").ap()

    x = din("x", [SEQ, D])
    pos = din("pos", [128, SEQ], I32)
    cst = din("cst", [128, NCST])
    bc = din("bc", [128, NBC])
    wF = din("wF", [NF, 128, 1024])
    wgd = din("wgd", [128, 128])
    wT = din("wT", [NT, 128, 4096])
    wbg = din("wbg", [8, 128, 1024])
    wbr = din("wbr", [8, 128, 2048])
    wo = din("wo", [2, 128, 4096])
    rw = din("rw", [128, 288])
    gku = din("gku", [16, 512])
    ewg = din("ewg", [4096, 4096])
    ewu = din("ewu", [4096, 4096])
    ewd = din("ewd", [4096, 4096])
    zr = din("zr", [512, D])
    out = nc.dram_tensor("out", [SEQ, D], F32, kind="ExternalOutput").ap()
    skind = "ExternalOutput" if debug else "Internal"
    o_all = nc.dram_tensor("o_all", [SEQ, 3072], F32, kind=skind).ap()
    h_all = nc.dram_tensor("h_all", [SEQ, D], F32, kind=skind).ap()
    hnT_all = nc.dram_tensor("hnT_all", [8, 128, SEQ], F32, kind=skind).ap()
    hn_all = nc.dram_tensor("hn_all", [SEQ, D], F32, kind=skind).ap()
    sg_all = nc.dram_tensor("sg_all", [16, 128, SEQ], F32, kind="Internal").ap()
    xs_d = nc.dram_tensor("xs_d", [8, D], F32, kind=skind).ap()
    inv_d = nc.dram_tensor("inv_d", [NSLOT, 16], I32, kind="Internal").ap()
    tokid = din("tokid", [128, 32 * 16], I32)
    zi = din("zi", [NSLOT, 16], I32)
    ys_d = nc.dram_tensor("ys_d", [NSLOT, D], F32, kind=skind).ap()
    c_dbg = nc.dram_tensor("c_dbg", [128, 1024], F32, kind=skind).ap()

    stack = ExitStack()
    arena_n = stack.enter_context(nc.sbuf_tensor("arenaN", [128, NCOLS_N], F32))
    arena_r = stack.enter_context(nc.sbuf_tensor("arenaR", [128, NCOLS_R], F32R))
    psum_t = stack.enter_context(nc.psum_tensor("ps", [128, 4096], F32))

    def A(c0, n):
        return arena_n[:, c0:c0 + n]

    def AR(c0, n):
        return arena_r[:, c0:c0 + n].bitcast(F32)

    top = [0]
    topR = [0]

    def alloc(n, align=64):
        c0 = (top[0] + align - 1) // align * align
        top[0] = c0 + n
        assert top[0] <= NCOLS_N, ("arenaN overflow", top[0])
        return c0

    def allocR(n, align=64):
        c0 = (topR[0] + align - 1) // align * align
        topR[0] = c0 + n
        assert topR[0] <= NCOLS_R, ("arenaR overflow", topR[0])
        return c0

    def r_(ap):
        return ap.bitcast(F32R)

    def MM(o, lhsT, rhs, start, stop, fast=True):
        if fast:
            l2, r2 = r_(lhsT), r_(rhs)
        else:
            l2, r2 = lhsT, rhs
        P.add("pe", lambda e: e.matmul(o, l2, r2, start=start, stop=stop), [lhsT, rhs], [o])
        if not fast:
            P.labels["pe"][-1] += "*"


    def TR(o, in_, idn):
        P.add("pe", lambda e: e.transpose(o, in_, idn), [in_, idn], [o])

    def ACT(o, in_, func, bias=None, scale=None):
        kw = {}
        reads = [in_]
        if bias is not None:
            kw["bias"] = bias
            if not isinstance(bias, (int, float)):
                reads.append(bias)
        if scale is not None:
            kw["scale"] = scale
            if not isinstance(scale, (int, float)):
                reads.append(scale)
        if func == AF.Copy and scale is not None and not isinstance(scale, (int, float)):
            func = AF.Identity
        P.add("act", lambda e: e.activation(out=o, in_=in_, func=func, **kw), reads, [o])

    def TT(eng, o, a, b, op):
        P.add(eng, lambda e: e.tensor_tensor(out=o, in0=a, in1=b, op=op), [a, b], [o])

    def TS(eng, o, a, s1, op0, s2=None, op1=None):
        reads = [a]
        if not isinstance(s1, (int, float)):
            reads.append(s1)
        if s2 is not None and not isinstance(s2, (int, float)):
            reads.append(s2)
        if op1 is None:
            P.add(eng, lambda e: e.tensor_scalar(out=o, in0=a, scalar1=s1, scalar2=None, op0=op0), reads, [o])
        else:
            P.add(eng, lambda e: e.tensor_scalar(out=o, in0=a, scalar1=s1, scalar2=s2, op0=op0, op1=op1), reads, [o])

    def STT(o, a, s, b, op0, op1, eng="dve"):
        reads = [a, b]
        if not isinstance(s, (int, float)):
            reads.append(s)
        P.add(eng, lambda e: e.scalar_tensor_tensor(out=o, in0=a, scalar=s, in1=b, op0=op0, op1=op1), reads, [o])

    def CP(eng, o, a):
        P.add(eng, lambda e: e.tensor_copy(out=o, in_=a), [a], [o])

    def DMA(eng, o, in_):
        P.add(eng, lambda e: e.dma_start(out=o, in_=in_), [in_], [o], dma=True)

    def DMAR(eng, o, in_):
        o2, i2 = r_(o), r_(in_)
        P.add(eng, lambda e: e.dma_start(out=o2, in_=i2), [in_], [o], dma=True)

    def MEMSET(eng, o, val):
        P.add(eng, lambda e: e.memset(o, val), [], [o])

    bank_i = [0]

    def bank():
        b = bank_i[0]
        bank_i[0] = (b + 1) % 8
        return psum_t[:, b * 512:(b + 1) * 512]

    c_cst = alloc(NCST)
    cstS = A(c_cst, NCST)
    gmask = A(c_cst + C_GM, 128)

    def rmask(h):
        return A(c_cst + C_RM + h * 128, 128)

    def rvec(i):
        return A(c_cst + C_RV + i, 1)

    theta = A(c_cst + C_TH, 1)

    def nmw(k):
        return A(c_cst + C_NMW + k, 1)

    def nfw(k):
        return A(c_cst + C_NFW + k, 1)

    c_ones = alloc(128)
    ones = A(c_ones, 128)
    c_ngkb = alloc(64)
    c_call = alloc(1024)
    sel_all = A(c_call, 1024).rearrange("p (t e) -> p t e", e=32)
    c_oh1 = alloc(1024)
    oh1_all = A(c_oh1, 1024).rearrange("p (t e) -> p t e", e=32)
    c_w12 = alloc(64)
    w12 = A(c_w12, 64)
    Lst = A(c_cst + C_LS, 128)
    bstart = A(c_cst + C_BS, NB)
    NSM = 12
    c_sm = alloc(64 * NSM)
    sm_i = [0]

    def sm(n=1):
        i = sm_i[0]
        sm_i[0] = (i + 1) % NSM
        return A(c_sm + 64 * i, n)

    pers_mark = top[0]
    c_gku = alloc(512)
    gkuS = arena_n[0:16, c_gku:c_gku + 512]
    c_rw = alloc(320)
    rwS = A(c_rw, 288).rearrange("p (k n) -> p k n", n=36)
    c_gnw = alloc(256)
    gnwS = A(c_gnw, 256)
    c_rb = alloc(64)
    rbS = A(c_rb, 36)
    NSP = 16
    c_sp = alloc(64 * NSP)
    negone = A(alloc(64), 1)
    job_mark = top[0]

    c_id = allocR(128)
    ident = AR(c_id, 128)
    persR_mark = topR[0]
    c_Sg = allocR(1024)
    c_Sr = allocR(4096)

    def Sg(h):
        return AR(c_Sg + h * 256, 256)

    def Sr(h, dt):
        return AR(c_Sr + (h * 2 + dt) * 512, 512)

    c_ws = allocR(8192)
    c_uT = allocR(8192)
    uT = AR(c_uT, 8192).rearrange("p (k t) -> p k t", k=8)
    ws_i = [0]

    def ws_alloc(nslots):
        i = ws_i[0]
        if i + nslots > 8:
            i = 0
        ws_i[0] = (i + nslots) % 8
        return AR(c_ws + i * 1024, nslots * 1024)

    jobR_mark = topR[0]

    zfill = []

    def zpop(n=1):
        for _ in range(n):
            if zfill:
                zfill.pop(0)()
    DMA("sp", cstS, cst)
    DMAR("sp", ident, cst[:, C_ID:C_ID + 128])
    DMA("sp", gkuS, gku)
    DMA("sp", A(c_rw, 288), rw)
    DMA("sp", gnwS, bc[:, B_GNW:B_GNW + 256])
    DMA("sp", rbS, bc[:, B_RB:B_RB + 36])
    MEMSET("dve", ones, 1.0)
    for i_ in range(2):
        TS("dve", r_(AR(c_Sg + i_ * 512, 512)), cstS[:, 0:512], 0.0, ALU.mult)
    for i_ in range(8):
        TS("dve", r_(AR(c_Sr + i_ * 512, 512)), cstS[:, 0:512], 0.0, ALU.mult)
    ngkb = A(c_ngkb, 4)
    TS("dve", ngkb, A(c_cst + C_GKB, 4), -1.0, ALU.mult)

    PI = math.pi
    TWO_PI = 2.0 * math.pi
    CW1 = 6.28125
    CW2 = TWO_PI - CW1
    PIC = 3.14159

    def rstd_from_stats(src_list, n_eps=EPS):
        st = sm(6 * len(src_list))
        for i, s in enumerate(src_list):
            si = st[:, 6 * i:6 * i + 6]
            P.add("dve", (lambda si, s: (lambda e: e.bn_stats(out=si, in_=s)))(si, s), [s], [si])
        mv = sm(2)
        P.add("dve", lambda e: e.bn_aggr(out=mv, in_=st), [st], [mv])
        t = sm(1)
        STT(t, mv[:, 0:1], mv[:, 0:1], mv[:, 1:2], ALU.mult, ALU.add)
        t2 = sm(1)
        TS("dve", t2, t, n_eps, ALU.add)
        sq = sm(1)
        ACT(sq, t2, AF.Sqrt)
        r = sm(1)
        P.add("dve", lambda e: e.reciprocal(out=r, in_=sq), [sq], [r])
        return r

    rot_steps = []

    def phase_A(q):
        P.label = "A%d" % q
        top[0] = job_mark
        topR[0] = jobR_mark
        t0 = q * QT
        cs = A(alloc(1024), 1024)
        sn = A(alloc(1024), 1024)
        mark = top[0]
        xb = [A(alloc(1024), 1024) for _ in range(2)]
        xs = [AR(allocR(1024), 1024) for _ in range(2)]

        def st1(tt):
            xt = xb[tt % 2]
            DMA("sp", xt, x[t0 + tt * 128:t0 + (tt + 1) * 128, :])
            r = rstd_from_stats([xt[:, 0:512], xt[:, 512:1024]])
            ACT(r_(xs[tt % 2]), xt, AF.Copy, scale=r)

        def st2(tt):
            xst = xs[tt % 2]
            for g in range(2):
                b = bank()
                for k4 in range(4):
                    kc = g * 4 + k4
                    TR(b[:, k4 * 128:(k4 + 1) * 128], xst[:, kc * 128:(kc + 1) * 128], ident)
                for k4 in range(4):
                    kc = g * 4 + k4
                    dst = uT[:, kc, tt * 128:(tt + 1) * 128]
                    if k4 % 2 == 0:
                        TS("dve", r_(dst), b[:, k4 * 128:(k4 + 1) * 128], nmw(kc), ALU.mult)
                    else:
                        ACT(r_(dst), b[:, k4 * 128:(k4 + 1) * 128], AF.Copy, scale=nmw(kc))

        st1(0)
        for tt in range(8):
            if tt + 1 < 8:
                st1(tt + 1)
            st2(tt)
        B0, B1, B2, B3 = cs, sn, xb[0], xb[1]
        PE_ = "dve"
        posi = B2.bitcast(I32)
        DMA("sp", posi, pos[:, t0:t0 + QT])
        steps = [
            lambda: CP(PE_, B0, posi),
            lambda: TS(PE_, B0, B0, theta, ALU.mult),
            lambda: TS(PE_, B1, B0, 1.0 / TWO_PI, ALU.mult),
            lambda: CP(PE_, B2.bitcast(I32), B1),
            lambda: CP(PE_, B1, B2.bitcast(I32)),
            lambda: TS(PE_, B3, B1, -CW1, ALU.mult),
            lambda: TT(PE_, B3, B3, B0, ALU.add),
            lambda: TS(PE_, B0, B1, -CW2, ALU.mult),
            lambda: TT(PE_, B3, B3, B0, ALU.add),
            lambda: TS(PE_, B0, B3, PI, ALU.is_gt, -TWO_PI, ALU.mult),
            lambda: TT(PE_, B3, B3, B0, ALU.add),
            lambda: TS(PE_, B0, B3, -PI, ALU.is_lt, TWO_PI, ALU.mult),
            lambda: TT(PE_, B3, B3, B0, ALU.add),
            lambda: TS(PE_, B1, B3, PIC, ALU.min, -PIC, ALU.max),
            lambda: ACT(sn, B1, AF.Sin),
            lambda: TS(PE_, B3, B3, PI / 2, ALU.add),
            lambda: TS(PE_, B0, B3, PI, ALU.is_gt, -TWO_PI, ALU.mult),
            lambda: TT(PE_, B3, B3, B0, ALU.add),
            lambda: TS(PE_, B0, B3, PIC, ALU.min, -PIC, ALU.max),
            lambda: ACT(cs, B0, AF.Sin),
        ]
        for st_ in steps:
            st_()
        top[0] = mark + 2048
        topR[0] = jobR_mark
        return cs, sn, mark

    def load_F(slab):
        w = ws_alloc(1)
        DMAR("sp", w, wF[slab])
        return w.rearrange("p (k m) -> p k m", k=8)

    def load_T(grp):
        w = ws_alloc(4)
        DMAR("sp", w, wT[grp])
        return w.rearrange("p (k n) -> p k n", k=8)

    def proj_F(w3, c0, b, n=512):
        for kc in range(8):
            MM(b[:, 0:n], w3[:, kc, :], uT[:, kc, c0:c0 + n], kc == 0, kc == 7)

    def proj_T(w3, tt, b):
        for kc in range(8):
            MM(b, uT[:, kc, tt * 128:(tt + 1) * 128], w3[:, kc, :], kc == 0, kc == 7)

    def job_gdown(q):
        c = alloc(1024)
        gdT = arena_n[0:16, c:c + 1024]
        c2 = alloc(128)
        w = A(c2, 128)
        DMA("sp", w, wgd)
        w3 = w.rearrange("p (k m) -> p k m", k=8)
        return gdT, w3

    def job_gla(q, h, gdT):
        P.label = "gla%d.%d" % (q, h)
        zpop(1)
        mark = top[0]
        markR = topR[0]
        t0 = q * QT
        qT = AR(allocR(1024), 1024)
        kT = AR(allocR(1024), 1024)
        v_sb = AR(allocR(2048), 2048).rearrange("p (t v) -> p t v", t=8)
        khT = [AR(allocR(128), 128) for _ in range(2)]
        ktok = [AR(allocR(128), 128) for _ in range(4)]
        sT = [AR(allocR(128), 128) for _ in range(4)]
        spv = A(alloc(1024), 1024)
        c_cum = alloc(1024)
        cum = A(c_cum, 1024)
        ecum = A(alloc(1024), 1024)
        encum = spv
        sgw = A(alloc(2048), 2048).rearrange("p (t v) -> p t v", t=8)
        oo = [A(c_cum + 256 * j, 256) for j in range(4)]
        etmp = A(c_cum, 512)
        for slab, dst, scale in ((F_GQ[h], qT, 128.0 ** -0.5), (F_GK[h], kT, 1.0)):
            w3 = load_F(slab)
            for half in range(2):
                b = bank()
                proj_F(w3, half * 512, b)
                ACT(r_(dst[:, half * 512:(half + 1) * 512]), b, AF.Copy, scale=scale)
        for half in range(2):
            b = bank()
            MM(b, gkuS[:, h * 128:(h + 1) * 128], gdT[:, half * 512:(half + 1) * 512], True, True, fast=False)
            ACT(etmp, b, AF.Exp, bias=ngkb[:, h:h + 1], scale=-1.0)
            ACT(spv[:, half * 512:(half + 1) * 512], etmp, AF.Ln, bias=1.0)
        for c in range(8):
            sl = slice(c * 128, (c + 1) * 128)
            o_, d1 = cum[:, sl], spv[:, sl]
            P.add("dve", (lambda o_, d1: (lambda e: e.tensor_tensor_scan(out=o_, data0=ones, data1=d1, initial=0.0,
                                                                        op0=ALU.mult, op1=ALU.add)))(o_, d1),
                  [ones, d1], [o_])
        ACT(ecum, cum, AF.Exp, scale=-1.0 / 16.0)
        ACT(encum, cum, AF.Exp, scale=1.0 / 16.0)
        TT("dve", r_(qT), qT, ecum, ALU.mult)
        TT("dve", r_(kT), kT, encum, ALU.mult)
        w3 = load_T(T_GLA[h])
        for tt in range(8):
            b = bank()
            proj_T(w3, tt, b)
            CP("dve", r_(v_sb[:, tt, :]), b[:, 0:256])
            ACT(sgw[:, tt, :], b[:, 256:512], AF.Silu)
            TT("dve", sgw[:, tt, :], sgw[:, tt, :], gnwS, ALU.mult)
        def gla_stage_a(c):
            sl = slice(c * 128, (c + 1) * 128)
            ecl = ecum[:, c * 128 + 127:c * 128 + 128]
            kh = khT[c % 2]
            kt = ktok[c % 4]
            s_ = sT[c % 4]
            TS("dve", r_(kh), kT[:, sl], ecl, ALU.mult)
            b = bank()
            TR(b[:, 0:128], kh, ident)
            ACT(r_(kt), b[:, 0:128], AF.Copy)
            b2 = bank()
            MM(b2[:, 0:128], kT[:, sl], qT[:, sl], True, True)
            TT("dve", r_(s_), b2[:, 0:128], gmask, ALU.mult)

        LA = 2
        pend_tail = []
        for c in range(LA):
            gla_stage_a(c)
        for c in range(8):
            if c + LA < 8:
                gla_stage_a(c + LA)
            sl = slice(c * 128, (c + 1) * 128)
            ecl = ecum[:, c * 128 + 127:c * 128 + 128]
            kt = ktok[c % 4]
            s_ = sT[c % 4]
            o_ = oo[c % 4]
            b3 = bank()
            MM(b3[:, 0:256], s_, v_sb[:, c, :], True, False)
            MM(b3[:, 0:256], qT[:, sl], Sg(h), False, True)
            b4 = bank()
            MM(b4[:, 0:256], kt, v_sb[:, c, :], True, True)
            STT(r_(Sg(h)), Sg(h), ecl, b4[:, 0:256], ALU.mult, ALU.add)
            if pend_tail:
                pend_tail.pop(0)()
            st = sm(6)
            src = b3[:, 0:256]
            P.add("dve", (lambda st, src: (lambda e: e.bn_stats(out=st, in_=src)))(st, src), [src], [st])
            mv = sm(2)
            P.add("dve", (lambda mv, st: (lambda e: e.bn_aggr(out=mv, in_=st)))(mv, st), [st], [mv])
            t = sm(1)
            STT(t, mv[:, 0:1], mv[:, 0:1], mv[:, 1:2], ALU.mult, ALU.add)
            t2 = sm(1)
            TS("dve", t2, t, EPS, ALU.add)
            sq = sm(1)
            ACT(sq, t2, AF.Sqrt)

            def tail(sq=sq, b3=b3, o_=o_, c=c):
                r = sm(1)
                P.add("dve", (lambda r, sq: (lambda e: e.reciprocal(out=r, in_=sq)))(r, sq), [sq], [r])
                STT(o_, b3[:, 0:256], r, sgw[:, c, :], ALU.mult, ALU.mult)
                DMA("pool", o_all[t0 + c * 128:t0 + (c + 1) * 128, h * 256:(h + 1) * 256], o_)

            pend_tail.append(tail)
        while pend_tail:
            pend_tail.pop(0)()
        top[0] = mark
        topR[0] = markR

    def job_ret(q, h, cs, sn):
        P.label = "ret%d.%d" % (q, h)
        zpop(1)
        mark = top[0]
        markR = topR[0]
        t0 = q * QT
        gam = 1.0 - 2.0 ** (-5.0 - h)
        gamC = gam ** 128
        qT = [AR(allocR(1024), 1024) for _ in range(2)]
        kT = [AR(allocR(1024), 1024) for _ in range(2)]
        v_sb = AR(allocR(4096), 4096).rearrange("p (t v) -> p t v", t=8)
        ktok = [AR(allocR(256), 256) for _ in range(4)]
        sT = [AR(allocR(128), 128) for _ in range(4)]
        sgw = A(alloc(4096), 4096).rearrange("p (t v) -> p t v", t=8)
        rnw_h = A(alloc(512), 512)
        tmpo = [A(alloc(512), 512) for _ in range(2)]
        t1 = [A(alloc(512), 512) for _ in range(4)]
        DMA("sp", rnw_h, bc[:, B_RNW + h * 512:B_RNW + (h + 1) * 512])
        for se, so, dst, scale in ((F_RQE[h], F_RQO[h], qT, 1.0), (F_RKE[h], F_RKO[h], kT, 1.0 / 16.0)):
            w3e = load_F(se)
            w3o = load_F(so)
            for half in range(2):
                hs = slice(half * 512, (half + 1) * 512)
                be = bank()
                proj_F(w3e, half * 512, be)
                bo = bank()
                proj_F(w3o, half * 512, bo)
                STT(t1[0], be, scale, cs[:, hs], ALU.mult, ALU.mult)
                STT(t1[1], bo, scale, sn[:, hs], ALU.mult, ALU.mult)
                STT(t1[2], bo, scale, cs[:, hs], ALU.mult, ALU.mult)
                STT(t1[3], be, scale, sn[:, hs], ALU.mult, ALU.mult)
                TT("pool", r_(dst[0][:, hs]), t1[0], t1[1], ALU.subtract)
                TT("pool", r_(dst[1][:, hs]), t1[2], t1[3], ALU.add)
        w3 = load_T(T_RV[h])
        for tt in range(8):
            b = bank()
            proj_T(w3, tt, b)
            ACT(r_(v_sb[:, tt, :]), b, AF.Copy)
        w3 = load_T(T_RG[h])
        for tt in range(8):
            b = bank()
            proj_T(w3, tt, b)
            ACT(sgw[:, tt, :], b, AF.Silu)
            TT("pool", sgw[:, tt, :], sgw[:, tt, :], rnw_h, ALU.mult)
        qd = rvec(h)
        qd2 = rvec(4 + h)
        kd = rvec(8 + h)
        def ret_stage_a(c):
            sl = slice(c * 128, (c + 1) * 128)
            kt = ktok[c % 4]
            s_ = sT[c % 4]
            b = bank()
            for dt in range(2):
                TR(b[:, dt * 128:(dt + 1) * 128], kT[dt][:, sl], ident)
            ACT(r_(kt), b[:, 0:256], AF.Copy, scale=kd)
            b2 = bank()
            for dt in range(2):
                MM(b2[:, 0:128], kT[dt][:, sl], qT[dt][:, sl], dt == 0, dt == 1)
            TT("dve", r_(s_), b2[:, 0:128], rmask(h), ALU.mult)

        LA = 2
        pend_tail = []
        for c in range(LA):
            ret_stage_a(c)
        for c in range(8):
            if c + LA < 8:
                ret_stage_a(c + LA)
            sl = slice(c * 128, (c + 1) * 128)
            kt = ktok[c % 4]
            s_ = sT[c % 4]
            to = tmpo[c % 2]
            b3 = bank()
            MM(b3, s_, v_sb[:, c, :], True, False)
            for dt in range(2):
                MM(b3, qT[dt][:, sl], Sr(h, dt), False, dt == 1)
            for dt in range(2):
                b4 = bank()
                MM(b4, kt[:, dt * 128:(dt + 1) * 128], v_sb[:, c, :], True, True)
                STT(r_(Sr(h, dt)), Sr(h, dt), gamC, b4, ALU.mult, ALU.add)
            if pend_tail:
                pend_tail.pop(0)()
            st = sm(6)
            P.add("dve", (lambda st, b3: (lambda e: e.bn_stats(out=st, in_=b3)))(st, b3), [b3], [st])
            mv = sm(2)
            P.add("dve", (lambda mv, st: (lambda e: e.bn_aggr(out=mv, in_=st)))(mv, st), [st], [mv])
            t = sm(1)
            TS("dve", t, mv[:, 1:2], qd2, ALU.mult, EPS, ALU.add)
            sq = sm(1)
            ACT(sq, t, AF.Sqrt)

            def tail(sq=sq, mv=mv, b3=b3, to=to, c=c):
                r = sm(1)
                P.add("dve", (lambda r, sq: (lambda e: e.reciprocal(out=r, in_=sq)))(r, sq), [sq], [r])
                rs = sm(1)
                TT("dve", rs, r, qd, ALU.mult)
                nmr = sm(1)
                STT(nmr, mv[:, 0:1], -1.0, rs, ALU.mult, ALU.mult)
                ACT(to, b3, AF.Identity, bias=nmr, scale=rs)
                TT("pool", to, to, sgw[:, c, :], ALU.mult)
                DMA("pool", o_all[t0 + c * 128:t0 + (c + 1) * 128, 1024 + h * 512:1024 + (h + 1) * 512], to)

            tail()
        while pend_tail:
            pend_tail.pop(0)()
        top[0] = mark
        topR[0] = markR

    sp_i = [0]

    def smp(n=1):
        i = sp_i[0]
        sp_i[0] = (i + 1) % NSP
        return A(c_sp + 64 * i, n)

    MEMSET("dve", negone, -1.0)

    def routing(tile_idx, b):
        PL = "pool"
        L = smp(36)
        TT("dve", L, b[:, 0:36], rbS, ALU.add)
        gmax = smp(1)
        P.add("dve", lambda e: e.tensor_reduce(out=gmax, in_=L[:, 0:4], axis=AX.X, op=ALU.max), [L], [gmax])
        goh = smp(4)
        TS("dve", goh, L[:, 0:4], gmax, ALU.is_equal)
        pen = smp(4)
        TS("dve", pen, goh, 1e30, ALU.mult, -1e30, ALU.add)
        Em = smp(32)
        for g in range(4):
            TS("dve", Em[:, g * 8:(g + 1) * 8], L[:, 4 + g * 8:4 + (g + 1) * 8], pen[:, g:g + 1], ALU.add)
        v1 = smp(1)
        P.add("dve", lambda e: e.tensor_reduce(out=v1, in_=Em, axis=AX.X, op=ALU.max), [Em], [v1])
        oh1 = oh1_all[:, tile_idx, :]
        TS("dve", oh1, Em, v1, ALU.is_ge)
        Em2 = smp(32)
        STT(Em2, oh1, -1e30, Em, ALU.mult, ALU.add)
        v2 = smp(1)
        P.add("dve", lambda e: e.tensor_reduce(out=v2, in_=Em2, axis=AX.X, op=ALU.max), [Em2], [v2])
        TS("dve", sel_all[:, tile_idx, :], Em, v2, ALU.is_ge)
        ngmax = smp(1)
        TS(PL, ngmax, gmax, -1.0, ALU.mult)
        gex = smp(4)
        ACT(gex, L[:, 0:4], AF.Exp, bias=ngmax, scale=1.0)
        g2 = smp(2)
        TT(PL, g2, gex[:, 0:2], gex[:, 2:4], ALU.add)
        gsum = smp(1)
        TT(PL, gsum, g2[:, 0:1], g2[:, 1:2], ALU.add)
        nv1 = smp(1)
        TS(PL, nv1, v1, -1.0, ALU.mult)
        ex2 = smp(1)
        ACT(ex2, v2, AF.Exp, bias=nv1, scale=1.0)
        prod = smp(1)
        TS(PL, prod, ex2, 1.0, ALU.add)
        TT(PL, prod, prod, gsum, ALU.mult)
        rw_ = w12[:, 2 * tile_idx:2 * tile_idx + 1]
        TT(PL, rw_, prod, negone, ALU.pow)
        TT(PL, w12[:, 2 * tile_idx + 1:2 * tile_idx + 2], rw_, ex2, ALU.mult)

    def job_mg(q):
        P.label = "mg%d" % q
        zpop(2)
        mark = top[0]
        t0 = q * QT
        sgb = [A(alloc(512), 512) for _ in range(4)]
        n = 0
        for i in range(16):
            w3 = load_F(F_MGA[i] if i < 8 else F_MGB[i - 8])
            for half in range(2):
                b = bank()
                proj_F(w3, half * 512, b)
                sg = sgb[n % 4]
                n += 1
                ACT(sg, b, AF.Sigmoid)
                DMA("pool", sg_all[i, :, t0 + half * 512:t0 + (half + 1) * 512], sg)
        top[0] = mark

    def post(q):
        P.label = "post%d" % q
        top[0] = job_mark
        topR[0] = c_uT
        t0 = q * QT
        G = 512
        oT = AR(allocR(24 * G), 24 * G).rearrange("p (v t) -> p v t", v=24)
        mT = AR(allocR(8 * G), 8 * G).rearrange("p (k t) -> p k t", k=8)
        c_old = [allocR(1024) for _ in range(2)]
        old = [AR(c, 1024).rearrange("p (t c) -> p t c", t=4) for c in c_old]
        hsb2 = [AR(c_old[0], 1024), AR(c_old[1], 1024)]
        xb = [A(alloc(1024), 1024) for _ in range(4)]
        hnT2 = [A(alloc(1024), 1024).rearrange("p (k t) -> p k t", k=8) for _ in range(2)]
        sab = [A(alloc(G), G) for _ in range(4)]
        nfwb = A(alloc(1024), 1024)
        hntok = A(alloc(1024), 1024)
        DMA("sp", nfwb, bc[:, B_NFW:B_NFW + 1024])
        for g in range(QT // G):
            th0 = t0 + g * G
            P.label = "posta%d" % q
            for vg in range(12):
                ol = ws_alloc(1).rearrange("p (t c) -> p t c", t=4)
                DMAR("sp", ol, o_all[th0:th0 + G, vg * 256:(vg + 1) * 256].rearrange("(t p) c -> p t c", p=128))
                for v2 in range(2):
                    vc = vg * 2 + v2
                    b = bank()
                    for tt in range(4):
                        TR(b[:, tt * 128:(tt + 1) * 128], ol[:, tt, v2 * 128:(v2 + 1) * 128], ident)
                    if vc % 2 == 0:
                        CP("dve", r_(oT[:, vc, :]), b)
                    else:
                        ACT(r_(oT[:, vc, :]), b, AF.Copy)
            P.label = "postb%d" % q
            for fc in range(8):
                wa = ws_alloc(1)
                DMAR("sp", wa, wbg[fc])
                wa3 = wa.rearrange("p (k m) -> p k m", k=8)
                wb = ws_alloc(2)
                DMAR("sp", wb, wbr[fc])
                wb3 = wb.rearrange("p (k m) -> p k m", k=16)
                sa = sab[(fc % 2) * 2]
                sb_ = sab[(fc % 2) * 2 + 1]
                DMA("sp", sa, sg_all[fc, :, th0:th0 + G])
                DMA("sp", sb_, sg_all[8 + fc, :, th0:th0 + G])
                bA = bank()
                for vc in range(8):
                    MM(bA, wa3[:, vc, :], oT[:, vc, :], vc == 0, vc == 7)
                bB = bank()
                for vc in range(16):
                    MM(bB, wb3[:, vc, :], oT[:, 8 + vc, :], vc == 0, vc == 15)
                TT("dve", sa, bA, sa, ALU.mult)
                TT("dve", sb_, bB, sb_, ALU.mult)
                TT("pool", r_(mT[:, fc, :]), sa, sb_, ALU.add)
            P.label = "postc%d" % q
            for tt in range(4):
                trow = th0 + tt * 128
                DMA("sp", xb[tt], x[trow:trow + 128, :])
            for fh in range(2):
                wo_h = ws_alloc(4)
                DMAR("sp", wo_h, wo[fh])
                wo3 = wo_h.rearrange("p (k n) -> p k n", k=8)
                for tt in range(4):
                    b = bank()
                    for fc in range(8):
                        MM(b, mT[:, fc, tt * 128:(tt + 1) * 128], wo3[:, fc, :], fc == 0, fc == 7)
                    xh = xb[tt][:, fh * 512:(fh + 1) * 512]
                    TT("dve", xh, b, xh, ALU.add)
            P.label = "postd%d" % q

            def pd1(tt):
                xt = xb[tt]
                trow = th0 + tt * 128
                DMA("pool", h_all[trow:trow + 128, :], xt)
                r = rstd_from_stats([xt[:, 0:512], xt[:, 512:1024]])
                ACT(r_(hsb2[tt % 2]), xt, AF.Copy, scale=r)

            def pd2(tt):
                trow = th0 + tt * 128
                hsb = hsb2[tt % 2]
                hnTt = hnT2[tt % 2]
                for g2 in range(2):
                    b = bank()
                    for k4 in range(4):
                        kc = g2 * 4 + k4
                        TR(b[:, k4 * 128:(k4 + 1) * 128], hsb[:, kc * 128:(kc + 1) * 128], ident)
                    for k4 in range(4):
                        kc = g2 * 4 + k4
                        if k4 % 2 == 0:
                            TS("dve", hnTt[:, kc, :], b[:, k4 * 128:(k4 + 1) * 128], nfw(kc), ALU.mult)
                        else:
                            ACT(hnTt[:, kc, :], b[:, k4 * 128:(k4 + 1) * 128], AF.Copy, scale=nfw(kc))
                TT("pool", hntok, hsb, nfwb, ALU.mult)
                DMA("pool", hn_all[trow:trow + 128, :], hntok)
                b = bank()
                for kc in range(8):
                    MM(b[:, 0:36], hnTt[:, kc, :], rwS[:, kc, :], kc == 0, kc == 7, fast=False)
                routing(trow // 128, b)

            pd1(0)
            for tt in range(4):
                if tt + 1 < 4:
                    pd1(tt + 1)
                pd2(tt)
        top[0] = job_mark
        topR[0] = jobR_mark

    def moe_dynamic():
        zpop(1000)
        P.label = "disp"
        top[0] = pers_mark
        topR[0] = persR_mark
        cnt = A(alloc(64), 32)
        nb = A(alloc(64), 32)
        pend = A(alloc(64), 32)
        pstart = A(alloc(64), 32)
        destf = A(alloc(64), 64)
        dest_i = A(alloc(64), 64).bitcast(I32)
        bef = A(alloc(64), NB)
        be_i = A(alloc(64), NB).bitcast(I32)
        widxf = A(alloc(64), NB)
        widx = A(alloc(64), NB).bitcast(I32)
        tmp32 = [A(alloc(64), 32) for _ in range(4)]
        mark = top[0]
        rank_all = A(alloc(1024), 1024).rearrange("p (t e) -> p t e", e=32)
        cums = A(alloc(1024), 1024).rearrange("p (t e) -> p t e", e=32)
        MEMSET("dve", cums[:, 0, :], 0.0)
        for i in range(1, 32):
            TT("dve", cums[:, i, :], cums[:, i - 1, :], sel_all[:, i - 1, :], ALU.add)
        csum = A(alloc(64), 32)
        TT("dve", csum, cums[:, 31, :], sel_all[:, 31, :], ALU.add)
        bk = [bank(), bank()]
        for i in range(32):
            o = bk[i // 16][:, (i % 16) * 32:(i % 16 + 1) * 32]
            MM(o, Lst, sel_all[:, i, :], True, i == 0, fast=False)
            if i > 0:
                MM(o, ones, cums[:, i, :], False, True, fast=False)
        bc_ = bank()
        MM(bc_[:, 0:32], ones, csum, True, True, fast=False)
        CP("dve", rank_all[:, 0:16, :], bk[0].rearrange("p (t e) -> p t e", e=32))
        CP("dve", rank_all[:, 16:32, :], bk[1].rearrange("p (t e) -> p t e", e=32))
        CP("dve", cnt, bc_[:, 0:32])
        MEMSET("dve", nb, 0.0)
        for m in range((SEQ + BS - 1) // BS):
            STT(nb, cnt, float(m * BS), nb, ALU.is_gt, ALU.add)
        P.add("dve", lambda e: e.tensor_tensor_scan(out=pend, data0=ones[:, 0:32], data1=nb, initial=0.0,
                                                    op0=ALU.mult, op1=ALU.add), [ones, nb], [pend])
        TS("dve", pend, pend, float(BS), ALU.mult)
        STT(pstart, nb, -float(BS), pend, ALU.mult, ALU.add)
        for i in range(32):
            t_ = tmp32[0]
            p1 = tmp32[1]
            p2 = tmp32[2]
            d12 = sm(1)
            TT("dve", t_, rank_all[:, i, :], pstart, ALU.add)
            TT("dve", p1, oh1_all[:, i, :], t_, ALU.mult)
            TT("dve", p2, sel_all[:, i, :], t_, ALU.mult)
            d1 = destf[:, 2 * i:2 * i + 1]
            d2 = destf[:, 2 * i + 1:2 * i + 2]
            P.add("dve", (lambda d1, p1: (lambda e: e.tensor_reduce(out=d1, in_=p1, axis=AX.X, op=ALU.add)))(d1, p1), [p1], [d1])
            P.add("dve", (lambda d12, p2: (lambda e: e.tensor_reduce(out=d12, in_=p2, axis=AX.X, op=ALU.add)))(d12, p2), [p2], [d12])
            TT("dve", d2, d12, d1, ALU.subtract)
        CP("dve", dest_i, destf)
        MEMSET("dve", bef, 0.0)
        for e_ in range(32):
            STT(bef, bstart, pend[:, e_:e_ + 1], bef, ALU.is_ge, ALU.add)
        CP("dve", be_i, bef)
        STT(widxf, bef, 128.0, A(c_cst + C_PI, NB), ALU.mult, ALU.add)
        CP("dve", widx, widxf)
        if debug:
            DMA("sp", c_dbg[:, 0:64], destf)
            DMA("sp", c_dbg[:, 64:64 + NB], bef)
            DMA("sp", c_dbg[:, 128:192], w12)
            DMA("sp", c_dbg[:, 192:224], cnt)
        top[0] = mark
        tok_s = A(alloc(512), 512).bitcast(I32).rearrange("p (t c) -> p t c", c=16)
        DMA("sp", tok_s, tokid.rearrange("p (t c) -> p t c", c=16))
        DMA("sp", inv_d, zi)
        for i in range(32):
            for k in range(2):
                idx = dest_i[:, 2 * i + k:2 * i + k + 1]
                src = tok_s[:, i, :]
                P.add("pool", (lambda idx, src: (lambda e: e.indirect_dma_start(
                    out=inv_d, out_offset=bass.IndirectOffsetOnAxis(ap=idx, axis=0), in_=src, in_offset=None)))(idx, src),
                    [idx, src], [inv_d], dma=True, merge=True, after=[inv_d])
        c_ring = allocR(20480)
        ring_i = [0]

        def ring():
            i = ring_i[0]
            ring_i[0] = (i + 1) % 5
            return AR(c_ring + i * 4096, 4096)

        c_xT = allocR(8 * BS)
        xT = AR(c_xT, 8 * BS).rearrange("p (k t) -> p k t", k=8)
        hid = AR(allocR(4 * BS), 4 * BS).rearrange("p (k t) -> p k t", k=4)
        xrow = [AR(allocR(1024), 1024) for _ in range(ST)]
        ysb = [A(alloc(1024), 1024) for _ in range(4)]
        sgt = [A(alloc(BS), BS) for _ in range(2)]
        invS = [A(alloc(64), 16).bitcast(I32) for _ in range(8)]
        bcreg = {}

        def gather_w(b_, tab):
            o = ring()
            idx = widx[:, b_:b_ + 1]

            def fn(e, idx=idx, o=o, tab=tab):
                if "r" not in bcreg:
                    rg = e.alloc_register("wbound")
                    e.reg_mov(rg, 4095)
                    bcreg["r"] = rg
                return e.indirect_dma_start(out=r_(o), out_offset=None, in_=tab,
                                            in_offset=bass.IndirectOffsetOnAxis(ap=idx, axis=0),
                                            bounds_check=bcreg["r"], oob_is_err=False)

            P.add("pool", fn, [idx], [o], dma=True)
            return o

        def loads(b_):
            w2 = [gather_w(b_, ewg), gather_w(b_, ewu)]
            for st in range(ST):
                ixf = invS[(b_ * ST + st) % 8]
                DMA("sp", ixf, inv_d[b_ * BS + st * 128:b_ * BS + (st + 1) * 128, :])
                ix = ixf[:, 0:1]
                P.add("pool", (lambda ix, o: (lambda e: e.indirect_dma_start(
                    out=r_(o), out_offset=None, in_=hn_all, in_offset=bass.IndirectOffsetOnAxis(ap=ix, axis=0))))(ix, xrow[st]),
                    [ix, hn_all], [xrow[st]], dma=True)
            return w2

        P.label = "exp"
        nxt = loads(0)
        nxt_wd = gather_w(0, ewd)
        for b_ in range(NB):
            cur = nxt + [nxt_wd]
            wg3 = cur[0].rearrange("p (k n) -> p k n", k=8)
            wu3 = cur[1].rearrange("p (k n) -> p k n", k=8)
            wd3 = cur[2].rearrange("p (k n) -> p k n", k=4)
            for kc in range(8):
                bkk = bank()
                for st in range(ST):
                    TR(bkk[:, st * 128:(st + 1) * 128], xrow[st][:, kc * 128:(kc + 1) * 128], ident)
                if kc % 2 == 0:
                    CP("dve", r_(xT[:, kc, :]), bkk[:, 0:BS])
                else:
                    ACT(r_(xT[:, kc, :]), bkk[:, 0:BS], AF.Copy)
            if debug and b_ == 0:
                DMA("sp", hnT_all[0, :, :], cur[0])
                DMA("sp", hnT_all[1, :, :], cur[2])
                DMA("sp", hnT_all[2, :, 0:8 * BS], AR(c_xT, 8 * BS))
            if b_ + 1 < NB:
                nxt = loads(b_ + 1)
            for hc in range(4):
                bG = bank()
                for kc in range(8):
                    MM(bG[:, 0:BS], wg3[:, kc, hc * 128:(hc + 1) * 128], xT[:, kc, :], kc == 0, kc == 7)
                bU = bank()
                for kc in range(8):
                    MM(bU[:, 0:BS], wu3[:, kc, hc * 128:(hc + 1) * 128], xT[:, kc, :], kc == 0, kc == 7)
                sg = sgt[hc % 2]
                ACT(sg, bG[:, 0:BS], AF.Silu)
                TT("dve", r_(hid[:, hc, :]), sg, bU[:, 0:BS], ALU.mult)
            if b_ + 1 < NB:
                nxt_wd = gather_w(b_ + 1, ewd)
            for st in range(ST):
                yb_ = ysb[(b_ * ST + st) % 4]
                for fh in range(2):
                    bb = bank()
                    for hc in range(4):
                        MM(bb, hid[:, hc, st * 128:(st + 1) * 128], wd3[:, hc, fh * 512:(fh + 1) * 512], hc == 0, hc == 3)
                    if fh == 0:
                        CP("dve", yb_[:, 0:512], bb)
                    else:
                        ACT(yb_[:, 512:1024], bb, AF.Copy)
                DMA("act", ys_d[b_ * BS + st * 128:b_ * BS + (st + 1) * 128, :], yb_)
        P.label = "comb"
        top[0] = mark
        nfin = A(alloc(1024), 1024)
        DMA("sp", nfin, bc[:, B_NF:B_NF + 1024])
        yg = [A(alloc(1024), 1024) for _ in range(6)]
        hb = [A(alloc(1024), 1024) for _ in range(3)]
        sqs = {}

        def cb1(i):
            ht = hb[i % 3]
            DMA("sp", ht, h_all[i * 128:(i + 1) * 128, :])
            for k in range(2):
                idx = dest_i[:, 2 * i + k:2 * i + k + 1]
                yk = yg[(i % 3) * 2 + k]
                P.add("pool", (lambda idx, yk: (lambda e: e.indirect_dma_start(
                    out=yk, out_offset=None, in_=ys_d, in_offset=bass.IndirectOffsetOnAxis(ap=idx, axis=0))))(idx, yk),
                    [idx, ys_d], [yk], dma=True)
                STT(ht, yk, w12[:, 2 * i + k:2 * i + k + 1], ht, ALU.mult, ALU.add)
            st = sm(12)
            for j in range(2):
                sj = st[:, 6 * j:6 * j + 6]
                src = ht[:, j * 512:(j + 1) * 512]
                P.add("dve", (lambda sj, src: (lambda e: e.bn_stats(out=sj, in_=src)))(sj, src), [src], [sj])
            mv = sm(2)
            P.add("dve", (lambda mv, st: (lambda e: e.bn_aggr(out=mv, in_=st)))(mv, st), [st], [mv])
            t = sm(1)
            STT(t, mv[:, 0:1], mv[:, 0:1], mv[:, 1:2], ALU.mult, ALU.add)
            t2 = sm(1)
            TS("dve", t2, t, EPS, ALU.add)
            sq = sm(1)
            ACT(sq, t2, AF.Sqrt)
            sqs[i] = sq

        def cb2(i):
            ht = hb[i % 3]
            sq = sqs.pop(i)
            r = sm(1)
            P.add("dve", (lambda r, sq: (lambda e: e.reciprocal(out=r, in_=sq)))(r, sq), [sq], [r])
            STT(ht, ht, r, nfin, ALU.mult, ALU.mult)
            DMA("act", out[i * 128:(i + 1) * 128, :], ht)

        cb1(0)
        for i in range(32):
            if i + 1 < 32:
                cb1(i + 1)
            cb2(i)

    nq = NQ if stage >= 2 else 1
    for q in range(nq):
        cs, sn, nmark = phase_A(q)
        if stage >= 1:
            gdT, wgd3 = job_gdown(q)
            for half in range(2):
                b = bank()
                for kc in range(8):
                    MM(b[0:16, :], wgd3[:, kc, :], uT[:, kc, half * 512:(half + 1) * 512], kc == 0, kc == 7, fast=False)
                CP("dve", gdT[:, half * 512:(half + 1) * 512], b[0:16, :])
            for h in range(4):
                job_gla(q, h, gdT)
            while rot_steps:
                rot_steps.pop(0)()
            top[0] = nmark
            for h in range(4):
                job_ret(q, h, cs, sn)
        if stage >= 2:
            job_mg(q)
            post(q)
    if stage >= 3:
        moe_dynamic()
    if debug and stage < 2:
        DMA("pool", hnT_all[:, :, 0:QT].rearrange("k p t -> p k t"), uT)

    P.emit(nc, stack)
    stack.close()
    nc._prog_labels = P.labels
    return nc


def _f_layout(W):
    K, M = W.shape
    return np.ascontiguousarray(W.reshape(K // 128, 128, M // 128, 128).transpose(2, 1, 0, 3)).reshape(M // 128, 128, (K // 128) * 128)


def _t_layout(W):
    K, N = W.shape
    return np.ascontiguousarray(W.reshape(K // 128, 128, N // 512, 512).transpose(2, 1, 0, 3)).reshape(N // 512, 128, (K // 128) * 512)


def _e_layout(W):
    E, K, N = W.shape
    return np.ascontiguousarray(W.reshape(E, K // 128, 128, N).transpose(0, 2, 1, 3)).reshape(E * 128, (K // 128) * N)


def _consts():
    c = np.zeros((128, NCST), np.float64)
    c[:, C_ID:C_ID + 128] = np.eye(128)
    j = np.arange(128)[:, None]
    i = np.arange(128)[None, :]
    c[:, C_GM:C_GM + 128] = (j <= i)
    for h in range(4):
        gam = 1.0 - 2.0 ** (-5.0 - h)
        c[:, C_RM + h * 128:C_RM + (h + 1) * 128] = np.where(j <= i, gam ** (-(j + 1.0)), 0.0)
        c[:, C_RV + h] = gam ** (np.arange(128) + 1.0)
        c[:, C_RV + 4 + h] = gam ** (2.0 * (np.arange(128) + 1.0))
        c[:, C_RV + 8 + h] = gam ** (127.0 - np.arange(128))
    c[:, C_LS:C_LS + 128] = (j < i)
    c[:, C_BS:C_BS + NB] = np.arange(NB)[None, :] * float(BS)
    c[:, C_PI:C_PI + 64] = np.arange(128)[:, None]
    th = 1.0 / (np.float32(10000.0) ** np.linspace(0.0, 1.0, 128, dtype=np.float32))
    c[:, C_TH] = th.astype(np.float64)
    return c.astype(np.float32)


def prep_inputs(inputs):
    f = lambda a: np.asarray(a, dtype=np.float32)
    w_in = f(inputs["w_in"])[0]
    slabs = [None] * NF
    for h in range(4):
        slabs[F_GQ[h]] = w_in[:, O_GQ + h * 128:O_GQ + (h + 1) * 128]
        slabs[F_GK[h]] = w_in[:, O_GK + h * 128:O_GK + (h + 1) * 128]
        rq = w_in[:, O_RQ + h * 256:O_RQ + (h + 1) * 256]
        rk = w_in[:, O_RK + h * 256:O_RK + (h + 1) * 256]
        slabs[F_RQE[h]] = rq[:, 0::2]
        slabs[F_RQO[h]] = rq[:, 1::2]
        slabs[F_RKE[h]] = rk[:, 0::2]
        slabs[F_RKO[h]] = rk[:, 1::2]
    for i in range(8):
        slabs[F_MGA[i]] = w_in[:, O_MGA + i * 128:O_MGA + (i + 1) * 128]
        slabs[F_MGB[i]] = w_in[:, O_MGB + i * 128:O_MGB + (i + 1) * 128]
    wF = np.stack([_f_layout(np.ascontiguousarray(s))[0] for s in slabs])
    wgd = np.ascontiguousarray(w_in[:, O_GD:O_GD + 16].reshape(8, 128, 16).transpose(1, 0, 2)).reshape(128, 128)
    groups = [None] * NT
    for h in range(4):
        groups[T_GLA[h]] = np.concatenate([w_in[:, O_GV + h * 256:O_GV + (h + 1) * 256],
                                           w_in[:, O_GG + h * 256:O_GG + (h + 1) * 256]], axis=1)
        groups[T_RV[h]] = w_in[:, O_RV + h * 512:O_RV + (h + 1) * 512]
        groups[T_RG[h]] = w_in[:, O_RG + h * 512:O_RG + (h + 1) * 512]
    wT = np.stack([_t_layout(np.ascontiguousarray(g))[0] for g in groups])
    wbg = _f_layout(f(inputs["w_branch_gla"])[0])
    wbr = _f_layout(f(inputs["w_branch_ret"])[0])
    wo = _t_layout(f(inputs["w_out"])[0])
    rwf = np.concatenate([f(inputs["router_group_w"])[0],
                          f(inputs["router_expert_w"])[0].transpose(1, 0, 2).reshape(1024, 32)], axis=1)
    rw = np.ascontiguousarray(rwf.reshape(8, 128, 36).transpose(1, 0, 2)).reshape(128, 288)
    cst = _consts()
    cst[:, C_NMW:C_NMW + 8] = f(inputs["norm_mix_w"])[0].reshape(8, 128).T
    cst[:, C_NFW:C_NFW + 8] = f(inputs["norm_ffn_w"])[0].reshape(8, 128).T
    cst[:, C_GKB:C_GKB + 4] = f(inputs["gla_gk_bias"])[0].reshape(4, 128).T
    bcv = np.zeros((NBC,), np.float32)
    bcv[B_GNW:B_GNW + 256] = f(inputs["gla_norm_w"])[0]
    bcv[B_RNW:B_RNW + 2048] = f(inputs["ret_norm_w"])[0]
    bcv[B_NF:B_NF + 1024] = f(inputs["norm_final_w"])
    bcv[B_NFW:B_NFW + 1024] = f(inputs["norm_ffn_w"])[0]
    bcv[B_RB:B_RB + 4] = f(inputs["router_group_b"])[0]
    bcv[B_RB + 4:B_RB + 36] = f(inputs["router_expert_b"])[0].reshape(32)
    bcr = np.ascontiguousarray(np.broadcast_to(bcv[None, :], (128, NBC)))
    tok = (np.arange(32, dtype=np.int32)[None, :, None] * 128 + np.arange(128, dtype=np.int32)[:, None, None])
    tok = np.ascontiguousarray(np.broadcast_to(tok, (128, 32, 16))).reshape(128, 512)
    shared = dict(zr=np.zeros((512, D), np.float32), zi=np.zeros((NSLOT, 16), np.int32), tokid=tok, cst=cst, bc=bcr, wF=wF, wgd=wgd, wT=wT, wbg=wbg, wbr=wbr, wo=wo, rw=rw,
                  gku=f(inputs["gla_gk_up"])[0],
                  ewg=_e_layout(f(inputs["expert_w_gate"])[0]), ewu=_e_layout(f(inputs["expert_w_up"])[0]),
                  ewd=_e_layout(f(inputs["expert_w_down"])[0]))
    xs = f(inputs["x"])
    ps = np.asarray(inputs["positions"]).astype(np.int32)
    in_maps = []
    for c in range(NCORES):
        m = dict(shared)
        m["x"] = np.ascontiguousarray(xs[c])
        m["pos"] = np.ascontiguousarray(np.broadcast_to(ps[c][None, :], (128, SEQ)))
        in_maps.append(m)
    return in_maps


def kernel(**inputs):
    in_maps = prep_inputs(inputs)
    nc = build()
    res = run_bass_kernel_spmd(nc, in_maps, core_ids=list(range(NCORES)))
    return np.stack([np.asarray(r["out"], dtype=np.float32) for r in res.results], axis=0)
```

```python
import os
import math
from contextlib import ExitStack
import numpy as np
import concourse.bass as bass
import concourse.mybir as mybir
from concourse.bass_utils import run_bass_kernel_spmd

F32 = mybir.dt.float32
F32R = mybir.dt.float32r
I32 = mybir.dt.int32
AF = mybir.ActivationFunctionType
ALU = mybir.AluOpType
AX = mybir.AxisListType

NCORES = 8
KVAR = int(os.environ.get('KVAR', '0'))
KSUB = int(os.environ.get('KSUB', '9'))
SEQ = 4096
D = 1024
EPS = 1e-6
NQ = 4
QT = 1024
NCOLS_N = 17280
NCOLS_R = 31872

O_GQ, O_GK, O_GV, O_GG, O_GD = 0, 512, 1024, 2048, 3072
O_RQ, O_RK, O_RV, O_RG = 3088, 4112, 5136, 7184
O_MGA, O_MGB = 9232, 10256
D_IN = 11280

F_GQ = [2 * h for h in range(4)]
F_GK = [2 * h + 1 for h in range(4)]
F_RQE = [8 + 4 * h for h in range(4)]
F_RQO = [9 + 4 * h for h in range(4)]
F_RKE = [10 + 4 * h for h in range(4)]
F_RKO = [11 + 4 * h for h in range(4)]
F_MGA = [24 + i for i in range(8)]
F_MGB = [32 + i for i in range(8)]
NF = 40
T_GLA = [h for h in range(4)]
T_RV = [4 + h for h in range(4)]
T_RG = [8 + h for h in range(4)]
NT = 12

C_ID, C_GM, C_RM, C_RV, C_TH, C_NMW, C_NFW, C_GKB = 0, 128, 256, 768, 784, 800, 808, 816
C_LS, C_BS, C_PI = 832, 960, 1024
NCST = 1088
BS = 384
ST = BS // 128
NB = (8192 + 32 * (BS - 1) + BS - 1) // BS
NSLOT = NB * BS
B_GNW, B_RNW, B_NF, B_RB, B_NFW = 0, 256, 2304, 3328, 3392
NBC = 4416


def _esz(dt):
    return 4


class Prog:
    ENGS = ("pe", "act", "dve", "pool", "sp")
    NDMA = 8

    def __init__(self):
        self.ops = {e: [] for e in self.ENGS}
        self.cnt = {e: 0 for e in self.ENGS}
        self.seen = {e: {} for e in self.ENGS}
        self.lastw = {}
        self.lastw_plain = {}
        self.readers = {}
        self.dma_uses = {}
        self.dma_next = {e: 0 for e in self.ENGS}
        self.final = {}
        self.label = ""
        self.labels = {e: [] for e in self.ENGS}

    @staticmethod
    def atoms(ap):
        space = str(ap.space)
        name = ap.tensor.name
        pairs = [(int(s), int(c)) for s, c in ap.ap]
        off = int(ap.offset)
        if space in ("SB", "PSUM"):
            rowstep = pairs[0][0]
            col0 = off % rowstep if rowstep > 0 else off
            ext = sum((c - 1) * abs(s) for s, c in pairs[1:])
            g = 64 if space == "SB" else 512
            return [(name, i) for i in range(col0 // g, (col0 + ext) // g + 1)]
        ext = sum((c - 1) * abs(s) for s, c in pairs)
        g = 16384
        return [(name, i) for i in range(off // g, (off + ext) // g + 1)]

    def add(self, eng, fn, reads, writes, dma=False, merge=False, after=()):
        deps = {}

        def need(ev):
            if ev is None:
                return
            for k, v in ev.items():
                if deps.get(k, 0) < v:
                    deps[k] = v

        writes = list(writes) + [ap for ap in reads if str(ap.space) == "PSUM"]
        reads = [ap for ap in reads if str(ap.space) != "PSUM"]
        for ap in after:
            for a in self.atoms(ap):
                need(self.lastw_plain.get(a))
        ratoms = []
        for ap in reads:
            for a in self.atoms(ap):
                ratoms.append(a)
                need(self.lastw.get(a))
        watoms = []
        for ap in writes:
            for a in self.atoms(ap):
                watoms.append(a)
                if not merge:
                    need(self.lastw.get(a))
                need(self.readers.get(a))
        if dma:
            k = self.dma_next[eng]
            self.dma_next[eng] = (k + 1) % self.NDMA
            key = ("dma", eng, k)
            u = self.dma_uses.get(key, 0)
            if u > 0:
                need({key: 16 * u})
            self.dma_uses[key] = u + 1
            ev = (key, 16 * (u + 1))
        else:
            self.cnt[eng] += 1
            ev = (eng, self.cnt[eng])
        self.final[ev[0]] = ev[1]
        waits = []
        seen = self.seen[eng]
        for k, v in deps.items():
            if k == eng and eng == "pe":
                continue
            if seen.get(k, 0) >= v:
                continue
            seen[k] = v
            waits.append((k, v))
        self.ops[eng].append((waits, fn, ev, dma))
        self.labels[eng].append(self.label)
        for a in ratoms:
            r = self.readers.setdefault(a, {})
            if r.get(ev[0], 0) < ev[1]:
                r[ev[0]] = ev[1]
        for a in watoms:
            if merge:
                lw = self.lastw.setdefault(a, {})
                if lw.get(ev[0], 0) < ev[1]:
                    lw[ev[0]] = ev[1]
            else:
                self.lastw[a] = {ev[0]: ev[1]}
                self.lastw_plain[a] = {ev[0]: ev[1]}
            self.readers[a] = {}

    def emit(self, nc, stack):
        keys = list(self.final.keys())
        sems = {}
        for i, k in enumerate(keys):
            sems[k] = stack.enter_context(nc.semaphore("s%d" % i))
        block = stack.enter_context(nc.Block())

        def mk(name):
            def body(eng):
                for waits, fn, ev, dma in self.ops[name]:
                    for k, v in waits:
                        eng.wait_ge(sems[k], v)
                    ins = fn(eng)
                    ins.then_inc(sems[ev[0]], 16 if dma else 1)
                if name == "sp":
                    for k, v in self.final.items():
                        eng.wait_ge(sems[k], v)
            return body

        block.tensor(mk("pe"))
        block.scalar(mk("act"))
        block.vector(mk("dve"))
        block.gpsimd(mk("pool"))
        block.sync(mk("sp"))


def build(stage=99, debug=False):
    nc = bass.Bass("TRN2", target_bir_lowering=False)
    nc.dge_precook = False
    P = Prog()

    def din(name, shape, dt=F32):
        return nc.dram_tensor(name, list(shape), dt, kind="ExternalInput").ap()

    x = din("x", [SEQ, D])
    pos = din("pos", [128, SEQ], I32)
    cst = din("cst", [128, NCST])
    bc = din("bc", [128, NBC])
    wF = din("wF", [NF, 128, 1024])
    wgd = din("wgd", [128, 128])
    wT = din("wT", [NT, 128, 4096])
    wbg = din("wbg", [8, 128, 1024])
    wbr = din("wbr", [8, 128, 2048])
    wo = din("wo", [2, 128, 4096])
    rw = din("rw", [128, 288])
    gku = din("gku", [16, 512])
    ewg = din("ewg", [4096, 4096])
    ewu = din("ewu", [4096, 4096])
    ewd = din("ewd", [4096, 4096])
    zr = din("zr", [512, D])
    out = nc.dram_tensor("out", [SEQ, D], F32, kind="ExternalOutput").ap()
    skind = "ExternalOutput" if debug else "Internal"
    o_all = nc.dram_tensor("o_all", [SEQ, 3072], F32, kind=skind).ap()
    h_all = nc.dram_tensor("h_all", [SEQ, D], F32, kind=skind).ap()
    hnT_all = nc.dram_tensor("hnT_all", [8, 128, SEQ], F32, kind=skind).ap()
    hn_all = nc.dram_tensor("hn_all", [SEQ, D], F32, kind=skind).ap()
    sg_all = nc.dram_tensor("sg_all", [16, 128, SEQ], F32, kind="Internal").ap()
    xs_d = nc.dram_tensor("xs_d", [8, D], F32, kind=skind).ap()
    inv_d = nc.dram_tensor("inv_d", [NSLOT, 16], I32, kind="Internal").ap()
    tokid = din("tokid", [128, 32 * 16], I32)
    zi = din("zi", [NSLOT, 16], I32)
    ys_d = nc.dram_tensor("ys_d", [NSLOT, D], F32, kind=skind).ap()
    c_dbg = nc.dram_tensor("c_dbg", [128, 1024], F32, kind=skind).ap()

    stack = ExitStack()
    arena_n = stack.enter_context(nc.sbuf_tensor("arenaN", [128, NCOLS_N], F32))
    arena_r = stack.enter_context(nc.sbuf_tensor("arenaR", [128, NCOLS_R], F32R))
    psum_t = stack.enter_context(nc.psum_tensor("ps", [128, 4096], F32))

    def A(c0, n):
        return arena_n[:, c0:c0 + n]

    def AR(c0, n):
        return arena_r[:, c0:c0 + n].bitcast(F32)

    top = [0]
    topR = [0]

    def alloc(n, align=64):
        c0 = (top[0] + align - 1) // align * align
        top[0] = c0 + n
        assert top[0] <= NCOLS_N, ("arenaN overflow", top[0])
        return c0

    def allocR(n, align=64):
        c0 = (topR[0] + align - 1) // align * align
        topR[0] = c0 + n
        assert topR[0] <= NCOLS_R, ("arenaR overflow", topR[0])
        return c0

    def r_(ap):
        return ap.bitcast(F32R)

    def MM(o, lhsT, rhs, start, stop, fast=True):
        if fast:
            l2, r2 = r_(lhsT), r_(rhs)
        else:
            l2, r2 = lhsT, rhs
        P.add("pe", lambda e: e.matmul(o, l2, r2, start=start, stop=stop), [lhsT, rhs], [o])
        if not fast:
            P.labels["pe"][-1] += "*"


    def TR(o, in_, idn):
        P.add("pe", lambda e: e.transpose(o, in_, idn), [in_, idn], [o])

    def ACT(o, in_, func, bias=None, scale=None):
        kw = {}
        reads = [in_]
        if bias is not None:
            kw["bias"] = bias
            if not isinstance(bias, (int, float)):
                reads.append(bias)
        if scale is not None:
            kw["scale"] = scale
            if not isinstance(scale, (int, float)):
                reads.append(scale)
        if func == AF.Copy and scale is not None and not isinstance(scale, (int, float)):
            func = AF.Identity
        P.add("act", lambda e: e.activation(out=o, in_=in_, func=func, **kw), reads, [o])

    def TT(eng, o, a, b, op):
        P.add(eng, lambda e: e.tensor_tensor(out=o, in0=a, in1=b, op=op), [a, b], [o])

    def TS(eng, o, a, s1, op0, s2=None, op1=None):
        reads = [a]
        if not isinstance(s1, (int, float)):
            reads.append(s1)
        if s2 is not None and not isinstance(s2, (int, float)):
            reads.append(s2)
        if op1 is None:
            P.add(eng, lambda e: e.tensor_scalar(out=o, in0=a, scalar1=s1, scalar2=None, op0=op0), reads, [o])
        else:
            P.add(eng, lambda e: e.tensor_scalar(out=o, in0=a, scalar1=s1, scalar2=s2, op0=op0, op1=op1), reads, [o])

    def STT(o, a, s, b, op0, op1, eng="dve"):
        reads = [a, b]
        if not isinstance(s, (int, float)):
            reads.append(s)
        P.add(eng, lambda e: e.scalar_tensor_tensor(out=o, in0=a, scalar=s, in1=b, op0=op0, op1=op1), reads, [o])

    def CP(eng, o, a):
        P.add(eng, lambda e: e.tensor_copy(out=o, in_=a), [a], [o])

    def DMA(eng, o, in_):
        P.add(eng, lambda e: e.dma_start(out=o, in_=in_), [in_], [o], dma=True)

    def DMAR(eng, o, in_):
        o2, i2 = r_(o), r_(in_)
        P.add(eng, lambda e: e.dma_start(out=o2, in_=i2), [in_], [o], dma=True)

    def MEMSET(eng, o, val):
        P.add(eng, lambda e: e.memset(o, val), [], [o])

    bank_i = [0]

    def bank():
        b = bank_i[0]
        bank_i[0] = (b + 1) % 8
        return psum_t[:, b * 512:(b + 1) * 512]

    c_cst = alloc(NCST)
    cstS = A(c_cst, NCST)
    gmask = A(c_cst + C_GM, 128)

    def rmask(h):
        return A(c_cst + C_RM + h * 128, 128)

    def rvec(i):
        return A(c_cst + C_RV + i, 1)

    theta = A(c_cst + C_TH, 1)

    def nmw(k):
        return A(c_cst + C_NMW + k, 1)

    def nfw(k):
        return A(c_cst + C_NFW + k, 1)

    c_ones = alloc(128)
    ones = A(c_ones, 128)
    c_ngkb = alloc(64)
    c_call = alloc(1024)
    sel_all = A(c_call, 1024).rearrange("p (t e) -> p t e", e=32)
    c_oh1 = alloc(1024)
    oh1_all = A(c_oh1, 1024).rearrange("p (t e) -> p t e", e=32)
    c_w12 = alloc(64)
    w12 = A(c_w12, 64)
    Lst = A(c_cst + C_LS, 128)
    bstart = A(c_cst + C_BS, NB)
    NSM = 12
    c_sm = alloc(64 * NSM)
    sm_i = [0]

    def sm(n=1):
        i = sm_i[0]
        sm_i[0] = (i + 1) % NSM
        return A(c_sm + 64 * i, n)

    pers_mark = top[0]
    c_gku = alloc(512)
    gkuS = arena_n[0:16, c_gku:c_gku + 512]
    c_rw = alloc(320)
    rwS = A(c_rw, 288).rearrange("p (k n) -> p k n", n=36)
    c_gnw = alloc(256)
    gnwS = A(c_gnw, 256)
    c_rb = alloc(64)
    rbS = A(c_rb, 36)
    NSP = 16
    c_sp = alloc(64 * NSP)
    negone = A(alloc(64), 1)
    job_mark = top[0]

    c_id = allocR(128)
    ident = AR(c_id, 128)
    persR_mark = topR[0]
    c_Sg = allocR(1024)
    c_Sr = allocR(4096)

    def Sg(h):
        return AR(c_Sg + h * 256, 256)

    def Sr(h, dt):
        return AR(c_Sr + (h * 2 + dt) * 512, 512)

    c_ws = allocR(8192)
    c_uT = allocR(8192)
    uT = AR(c_uT, 8192).rearrange("p (k t) -> p k t", k=8)
    ws_i = [0]

    def ws_alloc(nslots):
        i = ws_i[0]
        if i + nslots > 8:
            i = 0
        ws_i[0] = (i + nslots) % 8
        return AR(c_ws + i * 1024, nslots * 1024)

    jobR_mark = topR[0]

    zfill = []

    def zpop(n=1):
        for _ in range(n):
            if zfill:
                zfill.pop(0)()
    DMA("sp", cstS, cst)
    DMAR("sp", ident, cst[:, C_ID:C_ID + 128])
    DMA("sp", gkuS, gku)
    DMA("sp", A(c_rw, 288), rw)
    DMA("sp", gnwS, bc[:, B_GNW:B_GNW + 256])
    DMA("sp", rbS, bc[:, B_RB:B_RB + 36])
    MEMSET("dve", ones, 1.0)
    for i_ in range(2):
        TS("dve", r_(AR(c_Sg + i_ * 512, 512)), cstS[:, 0:512], 0.0, ALU.mult)
    for i_ in range(8):
        TS("dve", r_(AR(c_Sr + i_ * 512, 512)), cstS[:, 0:512], 0.0, ALU.mult)
    ngkb = A(c_ngkb, 4)
    TS("dve", ngkb, A(c_cst + C_GKB, 4), -1.0, ALU.mult)

    PI = math.pi
    TWO_PI = 2.0 * math.pi
    CW1 = 6.28125
    CW2 = TWO_PI - CW1
    PIC = 3.14159

    def rstd_from_stats(src_list, n_eps=EPS):
        st = sm(6 * len(src_list))
        for i, s in enumerate(src_list):
            si = st[:, 6 * i:6 * i + 6]
            P.add("dve", (lambda si, s: (lambda e: e.bn_stats(out=si, in_=s)))(si, s), [s], [si])
        mv = sm(2)
        P.add("dve", lambda e: e.bn_aggr(out=mv, in_=st), [st], [mv])
        t = sm(1)
        STT(t, mv[:, 0:1], mv[:, 0:1], mv[:, 1:2], ALU.mult, ALU.add)
        t2 = sm(1)
        TS("dve", t2, t, n_eps, ALU.add)
        sq = sm(1)
        ACT(sq, t2, AF.Sqrt)
        r = sm(1)
        P.add("dve", lambda e: e.reciprocal(out=r, in_=sq), [sq], [r])
        return r

    rot_steps = []

    def phase_A(q):
        P.label = "A%d" % q
        top[0] = job_mark
        topR[0] = jobR_mark
        t0 = q * QT
        cs = A(alloc(1024), 1024)
        sn = A(alloc(1024), 1024)
        mark = top[0]
        xb = [A(alloc(1024), 1024) for _ in range(2)]
        xs = [AR(allocR(1024), 1024) for _ in range(2)]

        def st1(tt):
            xt = xb[tt % 2]
            DMA("sp", xt, x[t0 + tt * 128:t0 + (tt + 1) * 128, :])
            r = rstd_from_stats([xt[:, 0:512], xt[:, 512:1024]])
            ACT(r_(xs[tt % 2]), xt, AF.Copy, scale=r)

        def st2(tt):
            xst = xs[tt % 2]
            for g in range(2):
                b = bank()
                for k4 in range(4):
                    kc = g * 4 + k4
                    TR(b[:, k4 * 128:(k4 + 1) * 128], xst[:, kc * 128:(kc + 1) * 128], ident)
                for k4 in range(4):
                    kc = g * 4 + k4
                    dst = uT[:, kc, tt * 128:(tt + 1) * 128]
                    if k4 % 2 == 0:
                        TS("dve", r_(dst), b[:, k4 * 128:(k4 + 1) * 128], nmw(kc), ALU.mult)
                    else:
                        ACT(r_(dst), b[:, k4 * 128:(k4 + 1) * 128], AF.Copy, scale=nmw(kc))

        st1(0)
        for tt in range(8):
            if tt + 1 < 8:
                st1(tt + 1)
            st2(tt)
        B0, B1, B2, B3 = cs, sn, xb[0], xb[1]
        PE_ = "dve"
        posi = B2.bitcast(I32)
        DMA("sp", posi, pos[:, t0:t0 + QT])
        steps = [
            lambda: CP(PE_, B0, posi),
            lambda: TS(PE_, B0, B0, theta, ALU.mult),
            lambda: TS(PE_, B1, B0, 1.0 / TWO_PI, ALU.mult),
            lambda: CP(PE_, B2.bitcast(I32), B1),
            lambda: CP(PE_, B1, B2.bitcast(I32)),
            lambda: TS(PE_, B3, B1, -CW1, ALU.mult),
            lambda: TT(PE_, B3, B3, B0, ALU.add),
            lambda: TS(PE_, B0, B1, -CW2, ALU.mult),
            lambda: TT(PE_, B3, B3, B0, ALU.add),
            lambda: TS(PE_, B0, B3, PI, ALU.is_gt, -TWO_PI, ALU.mult),
            lambda: TT(PE_, B3, B3, B0, ALU.add),
            lambda: TS(PE_, B0, B3, -PI, ALU.is_lt, TWO_PI, ALU.mult),
            lambda: TT(PE_, B3, B3, B0, ALU.add),
            lambda: TS(PE_, B1, B3, PIC, ALU.min, -PIC, ALU.max),
            lambda: ACT(sn, B1, AF.Sin),
            lambda: TS(PE_, B3, B3, PI / 2, ALU.add),
            lambda: TS(PE_, B0, B3, PI, ALU.is_gt, -TWO_PI, ALU.mult),
            lambda: TT(PE_, B3, B3, B0, ALU.add),
            lambda: TS(PE_, B0, B3, PIC, ALU.min, -PIC, ALU.max),
            lambda: ACT(cs, B0, AF.Sin),
        ]
        for st_ in steps:
            st_()
        top[0] = mark + 2048
        topR[0] = jobR_mark
        return cs, sn, mark

    def load_F(slab):
        w = ws_alloc(1)
        DMAR("sp", w, wF[slab])
        return w.rearrange("p (k m) -> p k m", k=8)

    def load_T(grp):
        w = ws_alloc(4)
        DMAR("sp", w, wT[grp])
        return w.rearrange("p (k n) -> p k n", k=8)

    def proj_F(w3, c0, b, n=512):
        for kc in range(8):
            MM(b[:, 0:n], w3[:, kc, :], uT[:, kc, c0:c0 + n], kc == 0, kc == 7)

    def proj_T(w3, tt, b):
        for kc in range(8):
            MM(b, uT[:, kc, tt * 128:(tt + 1) * 128], w3[:, kc, :], kc == 0, kc == 7)

    def job_gdown(q):
        c = alloc(1024)
        gdT = arena_n[0:16, c:c + 1024]
        c2 = alloc(128)
        w = A(c2, 128)
        DMA("sp", w, wgd)
        w3 = w.rearrange("p (k m) -> p k m", k=8)
        return gdT, w3

    def job_gla(q, h, gdT):
        P.label = "gla%d.%d" % (q, h)
        zpop(1)
        mark = top[0]
        markR = topR[0]
        t0 = q * QT
        qT = AR(allocR(1024), 1024)
        kT = AR(allocR(1024), 1024)
        v_sb = AR(allocR(2048), 2048).rearrange("p (t v) -> p t v", t=8)
        khT = [AR(allocR(128), 128) for _ in range(2)]
        ktok = [AR(allocR(128), 128) for _ in range(4)]
        sT = [AR(allocR(128), 128) for _ in range(4)]
        spv = A(alloc(1024), 1024)
        c_cum = alloc(1024)
        cum = A(c_cum, 1024)
        ecum = A(alloc(1024), 1024)
        encum = spv
        sgw = A(alloc(2048), 2048).rearrange("p (t v) -> p t v", t=8)
        oo = [A(c_cum + 256 * j, 256) for j in range(4)]
        etmp = A(c_cum, 512)
        for slab, dst, scale in ((F_GQ[h], qT, 128.0 ** -0.5), (F_GK[h], kT, 1.0)):
            w3 = load_F(slab)
            for half in range(2):
                b = bank()
                proj_F(w3, half * 512, b)
                ACT(r_(dst[:, half * 512:(half + 1) * 512]), b, AF.Copy, scale=scale)
        for half in range(2):
            b = bank()
            MM(b, gkuS[:, h * 128:(h + 1) * 128], gdT[:, half * 512:(half + 1) * 512], True, True, fast=False)
            ACT(etmp, b, AF.Exp, bias=ngkb[:, h:h + 1], scale=-1.0)
            ACT(spv[:, half * 512:(half + 1) * 512], etmp, AF.Ln, bias=1.0)
        for c in range(8):
            sl = slice(c * 128, (c + 1) * 128)
            o_, d1 = cum[:, sl], spv[:, sl]
            P.add("dve", (lambda o_, d1: (lambda e: e.tensor_tensor_scan(out=o_, data0=ones, data1=d1, initial=0.0,
                                                                        op0=ALU.mult, op1=ALU.add)))(o_, d1),
                  [ones, d1], [o_])
        ACT(ecum, cum, AF.Exp, scale=-1.0 / 16.0)
        ACT(encum, cum, AF.Exp, scale=1.0 / 16.0)
        TT("dve", r_(qT), qT, ecum, ALU.mult)
        TT("dve", r_(kT), kT, encum, ALU.mult)
        w3 = load_T(T_GLA[h])
        for tt in range(8):
            b = bank()
            proj_T(w3, tt, b)
            CP("dve", r_(v_sb[:, tt, :]), b[:, 0:256])
            ACT(sgw[:, tt, :], b[:, 256:512], AF.Silu)
            TT("dve", sgw[:, tt, :], sgw[:, tt, :], gnwS, ALU.mult)
        def gla_stage_a(c):
            sl = slice(c * 128, (c + 1) * 128)
            ecl = ecum[:, c * 128 + 127:c * 128 + 128]
            kh = khT[c % 2]
            kt = ktok[c % 4]
            s_ = sT[c % 4]
            TS("dve", r_(kh), kT[:, sl], ecl, ALU.mult)
            b = bank()
            TR(b[:, 0:128], kh, ident)
            ACT(r_(kt), b[:, 0:128], AF.Copy)
            b2 = bank()
            MM(b2[:, 0:128], kT[:, sl], qT[:, sl], True, True)
            TT("dve", r_(s_), b2[:, 0:128], gmask, ALU.mult)

        LA = 2
        pend_tail = []
        for c in range(LA):
            gla_stage_a(c)
        for c in range(8):
            if c + LA < 8:
                gla_stage_a(c + LA)
            sl = slice(c * 128, (c + 1) * 128)
            ecl = ecum[:, c * 128 + 127:c * 128 + 128]
            kt = ktok[c % 4]
            s_ = sT[c % 4]
            o_ = oo[c % 4]
            b3 = bank()
            MM(b3[:, 0:256], s_, v_sb[:, c, :], True, False)
            MM(b3[:, 0:256], qT[:, sl], Sg(h), False, True)
            b4 = bank()
            MM(b4[:, 0:256], kt, v_sb[:, c, :], True, True)
            STT(r_(Sg(h)), Sg(h), ecl, b4[:, 0:256], ALU.mult, ALU.add)
            if pend_tail:
                pend_tail.pop(0)()
            st = sm(6)
            src = b3[:, 0:256]
            P.add("dve", (lambda st, src: (lambda e: e.bn_stats(out=st, in_=src)))(st, src), [src], [st])
            mv = sm(2)
            P.add("dve", (lambda mv, st: (lambda e: e.bn_aggr(out=mv, in_=st)))(mv, st), [st], [mv])
            t = sm(1)
            STT(t, mv[:, 0:1], mv[:, 0:1], mv[:, 1:2], ALU.mult, ALU.add)
            t2 = sm(1)
            TS("dve", t2, t, EPS, ALU.add)
            sq = sm(1)
            ACT(sq, t2, AF.Sqrt)

            def tail(sq=sq, b3=b3, o_=o_, c=c):
                r = sm(1)
                P.add("dve", (lambda r, sq: (lambda e: e.reciprocal(out=r, in_=sq)))(r, sq), [sq], [r])
                STT(o_, b3[:, 0:256], r, sgw[:, c, :], ALU.mult, ALU.mult)
                DMA("pool", o_all[t0 + c * 128:t0 + (c + 1) * 128, h * 256:(h + 1) * 256], o_)

            pend_tail.append(tail)
        while pend_tail:
            pend_tail.pop(0)()
        top[0] = mark
        topR[0] = markR

    def job_ret(q, h, cs, sn):
        P.label = "ret%d.%d" % (q, h)
        zpop(1)
        mark = top[0]
        markR = topR[0]
        t0 = q * QT
        gam = 1.0 - 2.0 ** (-5.0 - h)
        gamC = gam ** 128
        qT = [AR(allocR(1024), 1024) for _ in range(2)]
        kT = [AR(allocR(1024), 1024) for _ in range(2)]
        v_sb = AR(allocR(4096), 4096).rearrange("p (t v) -> p t v", t=8)
        ktok = [AR(allocR(256), 256) for _ in range(4)]
        sT = [AR(allocR(128), 128) for _ in range(4)]
        sgw = A(alloc(4096), 4096).rearrange("p (t v) -> p t v", t=8)
        rnw_h = A(alloc(512), 512)
        tmpo = [A(alloc(512), 512) for _ in range(2)]
        t1 = [A(alloc(512), 512) for _ in range(4)]
        DMA("sp", rnw_h, bc[:, B_RNW + h * 512:B_RNW + (h + 1) * 512])
        for se, so, dst, scale in ((F_RQE[h], F_RQO[h], qT, 1.0), (F_RKE[h], F_RKO[h], kT, 1.0 / 16.0)):
            w3e = load_F(se)
            w3o = load_F(so)
            for half in range(2):
                hs = slice(half * 512, (half + 1) * 512)
                be = bank()
                proj_F(w3e, half * 512, be)
                bo = bank()
                proj_F(w3o, half * 512, bo)
                STT(t1[0], be, scale, cs[:, hs], ALU.mult, ALU.mult)
                STT(t1[1], bo, scale, sn[:, hs], ALU.mult, ALU.mult)
                STT(t1[2], bo, scale, cs[:, hs], ALU.mult, ALU.mult)
                STT(t1[3], be, scale, sn[:, hs], ALU.mult, ALU.mult)
                TT("pool", r_(dst[0][:, hs]), t1[0], t1[1], ALU.subtract)
                TT("pool", r_(dst[1][:, hs]), t1[2], t1[3], ALU.add)
        w3 = load_T(T_RV[h])
        for tt in range(8):
            b = bank()
            proj_T(w3, tt, b)
            ACT(r_(v_sb[:, tt, :]), b, AF.Copy)
        w3 = load_T(T_RG[h])
        for tt in range(8):
            b = bank()
            proj_T(w3, tt, b)
            ACT(sgw[:, tt, :], b, AF.Silu)
            TT("pool", sgw[:, tt, :], sgw[:, tt, :], rnw_h, ALU.mult)
        qd = rvec(h)
        qd2 = rvec(4 + h)
        kd = rvec(8 + h)
        def ret_stage_a(c):
            sl = slice(c * 128, (c + 1) * 128)
            kt = ktok[c % 4]
            s_ = sT[c % 4]
            b = bank()
            for dt in range(2):
                TR(b[:, dt * 128:(dt + 1) * 128], kT[dt][:, sl], ident)
            ACT(r_(kt), b[:, 0:256], AF.Copy, scale=kd)
            b2 = bank()
            for dt in range(2):
                MM(b2[:, 0:128], kT[dt][:, sl], qT[dt][:, sl], dt == 0, dt == 1)
            TT("dve", r_(s_), b2[:, 0:128], rmask(h), ALU.mult)

        LA = 2
        pend_tail = []
        for c in range(LA):
            ret_stage_a(c)
        for c in range(8):
            if c + LA < 8:
                ret_stage_a(c + LA)
            sl = slice(c * 128, (c + 1) * 128)
            kt = ktok[c % 4]
            s_ = sT[c % 4]
            to = tmpo[c % 2]
            b3 = bank()
            MM(b3, s_, v_sb[:, c, :], True, False)
            for dt in range(2):
                MM(b3, qT[dt][:, sl], Sr(h, dt), False, dt == 1)
            for dt in range(2):
                b4 = bank()
                MM(b4, kt[:, dt * 128:(dt + 1) * 128], v_sb[:, c, :], True, True)
                STT(r_(Sr(h, dt)), Sr(h, dt), gamC, b4, ALU.mult, ALU.add)
            if pend_tail:
                pend_tail.pop(0)()
            st = sm(6)
            P.add("dve", (lambda st, b3: (lambda e: e.bn_stats(out=st, in_=b3)))(st, b3), [b3], [st])
            mv = sm(2)
            P.add("dve", (lambda mv, st: (lambda e: e.bn_aggr(out=mv, in_=st)))(mv, st), [st], [mv])
            t = sm(1)
            TS("dve", t, mv[:, 1:2], qd2, ALU.mult, EPS, ALU.add)
            sq = sm(1)
            ACT(sq, t, AF.Sqrt)

            def tail(sq=sq, mv=mv, b3=b3, to=to, c=c):
                r = sm(1)
                P.add("dve", (lambda r, sq: (lambda e: e.reciprocal(out=r, in_=sq)))(r, sq), [sq], [r])
                rs = sm(1)
                TT("dve", rs, r, qd, ALU.mult)
                nmr = sm(1)
                STT(nmr, mv[:, 0:1], -1.0, rs, ALU.mult, ALU.mult)
                ACT(to, b3, AF.Identity, bias=nmr, scale=rs)
                TT("pool", to, to, sgw[:, c, :], ALU.mult)
                DMA("pool", o_all[t0 + c * 128:t0 + (c + 1) * 128, 1024 + h * 512:1024 + (h + 1) * 512], to)

            tail()
        while pend_tail:
            pend_tail.pop(0)()
        top[0] = mark
        topR[0] = markR

    sp_i = [0]

    def smp(n=1):
        i = sp_i[0]
        sp_i[0] = (i + 1) % NSP
        return A(c_sp + 64 * i, n)

    MEMSET("dve", negone, -1.0)

    def routing(tile_idx, b):
        PL = "pool"
        L = smp(36)
        TT("dve", L, b[:, 0:36], rbS, ALU.add)
        gmax = smp(1)
        P.add("dve", lambda e: e.tensor_reduce(out=gmax, in_=L[:, 0:4], axis=AX.X, op=ALU.max), [L], [gmax])
        goh = smp(4)
        TS("dve", goh, L[:, 0:4], gmax, ALU.is_equal)
        pen = smp(4)
        TS("dve", pen, goh, 1e30, ALU.mult, -1e30, ALU.add)
        Em = smp(32)
        for g in range(4):
            TS("dve", Em[:, g * 8:(g + 1) * 8], L[:, 4 + g * 8:4 + (g + 1) * 8], pen[:, g:g + 1], ALU.add)
        v1 = smp(1)
        P.add("dve", lambda e: e.tensor_reduce(out=v1, in_=Em, axis=AX.X, op=ALU.max), [Em], [v1])
        oh1 = oh1_all[:, tile_idx, :]
        TS("dve", oh1, Em, v1, ALU.is_ge)
        Em2 = smp(32)
        STT(Em2, oh1, -1e30, Em, ALU.mult, ALU.add)
        v2 = smp(1)
        P.add("dve", lambda e: e.tensor_reduce(out=v2, in_=Em2, axis=AX.X, op=ALU.max), [Em2], [v2])
        TS("dve", sel_all[:, tile_idx, :], Em, v2, ALU.is_ge)
        ngmax = smp(1)
        TS(PL, ngmax, gmax, -1.0, ALU.mult)
        gex = smp(4)
        ACT(gex, L[:, 0:4], AF.Exp, bias=ngmax, scale=1.0)
        g2 = smp(2)
        TT(PL, g2, gex[:, 0:2], gex[:, 2:4], ALU.add)
        gsum = smp(1)
        TT(PL, gsum, g2[:, 0:1], g2[:, 1:2], ALU.add)
        nv1 = smp(1)
        TS(PL, nv1, v1, -1.0, ALU.mult)
        ex2 = smp(1)
        ACT(ex2, v2, AF.Exp, bias=nv1, scale=1.0)
        prod = smp(1)
        TS(PL, prod, ex2, 1.0, ALU.add)
        TT(PL, prod, prod, gsum, ALU.mult)
        rw_ = w12[:, 2 * tile_idx:2 * tile_idx + 1]
        TT(PL, rw_, prod, negone, ALU.pow)
        TT(PL, w12[:, 2 * tile_idx + 1:2 * tile_idx + 2], rw_, ex2, ALU.mult)

    def job_mg(q):
        P.label = "mg%d" % q
        zpop(2)
        mark = top[0]
        t0 = q * QT
        sgb = [A(alloc(512), 512) for _ in range(4)]
        n = 0
        for i in range(16):
            w3 = load_F(F_MGA[i] if i < 8 else F_MGB[i - 8])
            for half in range(2):
                b = bank()
                proj_F(w3, half * 512, b)
                sg = sgb[n % 4]
                n += 1
                ACT(sg, b, AF.Sigmoid)
                DMA("pool", sg_all[i, :, t0 + half * 512:t0 + (half + 1) * 512], sg)
        top[0] = mark

    def post(q):
        P.label = "post%d" % q
        top[0] = job_mark
        topR[0] = c_uT
        t0 = q * QT
        G = 512
        oT = AR(allocR(24 * G), 24 * G).rearrange("p (v t) -> p v t", v=24)
        mT = AR(allocR(8 * G), 8 * G).rearrange("p (k t) -> p k t", k=8)
        c_old = [allocR(1024) for _ in range(2)]
        old = [AR(c, 1024).rearrange("p (t c) -> p t c", t=4) for c in c_old]
        hsb2 = [AR(c_old[0], 1024), AR(c_old[1], 1024)]
        xb = [A(alloc(1024), 1024) for _ in range(4)]
        hnT2 = [A(alloc(1024), 1024).rearrange("p (k t) -> p k t", k=8) for _ in range(2)]
        sab = [A(alloc(G), G) for _ in range(4)]
        nfwb = A(alloc(1024), 1024)
        hntok = A(alloc(1024), 1024)
        DMA("sp", nfwb, bc[:, B_NFW:B_NFW + 1024])
        for g in range(QT // G):
            th0 = t0 + g * G
            P.label = "posta%d" % q
            for vg in range(12):
                ol = ws_alloc(1).rearrange("p (t c) -> p t c", t=4)
                DMAR("sp", ol, o_all[th0:th0 + G, vg * 256:(vg + 1) * 256].rearrange("(t p) c -> p t c", p=128))
                for v2 in range(2):
                    vc = vg * 2 + v2
                    b = bank()
                    for tt in range(4):
                        TR(b[:, tt * 128:(tt + 1) * 128], ol[:, tt, v2 * 128:(v2 + 1) * 128], ident)
                    if vc % 2 == 0:
                        CP("dve", r_(oT[:, vc, :]), b)
                    else:
                        ACT(r_(oT[:, vc, :]), b, AF.Copy)
            P.label = "postb%d" % q
            for fc in range(8):
                wa = ws_alloc(1)
                DMAR("sp", wa, wbg[fc])
                wa3 = wa.rearrange("p (k m) -> p k m", k=8)
                wb = ws_alloc(2)
                DMAR("sp", wb, wbr[fc])
                wb3 = wb.rearrange("p (k m) -> p k m", k=16)
                sa = sab[(fc % 2) * 2]
                sb_ = sab[(fc % 2) * 2 + 1]
                DMA("act", sa, sg_all[fc, :, th0:th0 + G])
                DMA("act", sb_, sg_all[8 + fc, :, th0:th0 + G])
                bA = bank()
                for vc in range(8):
                    MM(bA, wa3[:, vc, :], oT[:, vc, :], vc == 0, vc == 7)
                bB = bank()
                for vc in range(16):
                    MM(bB, wb3[:, vc, :], oT[:, 8 + vc, :], vc == 0, vc == 15)
                TT("dve", sa, bA, sa, ALU.mult)
                TT("dve", sb_, bB, sb_, ALU.mult)
                TT("pool", r_(mT[:, fc, :]), sa, sb_, ALU.add)
            P.label = "postc%d" % q
            for tt in range(4):
                trow = th0 + tt * 128
                DMA("sp", xb[tt], x[trow:trow + 128, :])
            for fh in range(2):
                wo_h = ws_alloc(4)
                DMAR("sp", wo_h, wo[fh])
                wo3 = wo_h.rearrange("p (k n) -> p k n", k=8)
                for tt in range(4):
                    b = bank()
                    for fc in range(8):
                        MM(b, mT[:, fc, tt * 128:(tt + 1) * 128], wo3[:, fc, :], fc == 0, fc == 7)
                    xh = xb[tt][:, fh * 512:(fh + 1) * 512]
                    TT("dve", xh, b, xh, ALU.add)
            P.label = "postd%d" % q

            def pd1(tt):
                xt = xb[tt]
                trow = th0 + tt * 128
                DMA("pool", h_all[trow:trow + 128, :], xt)
                r = rstd_from_stats([xt[:, 0:512], xt[:, 512:1024]])
                ACT(r_(hsb2[tt % 2]), xt, AF.Copy, scale=r)

            def pd2(tt):
                trow = th0 + tt * 128
                hsb = hsb2[tt % 2]
                hnTt = hnT2[tt % 2]
                for g2 in range(2):
                    b = bank()
                    for k4 in range(4):
                        kc = g2 * 4 + k4
                        TR(b[:, k4 * 128:(k4 + 1) * 128], hsb[:, kc * 128:(kc + 1) * 128], ident)
                    for k4 in range(4):
                        kc = g2 * 4 + k4
                        if k4 % 2 == 0:
                            TS("dve", hnTt[:, kc, :], b[:, k4 * 128:(k4 + 1) * 128], nfw(kc), ALU.mult)
                        else:
                            ACT(hnTt[:, kc, :], b[:, k4 * 128:(k4 + 1) * 128], AF.Copy, scale=nfw(kc))
                TT("pool", hntok, hsb, nfwb, ALU.mult)
                DMA("pool", hn_all[trow:trow + 128, :], hntok)
                b = bank()
                for kc in range(8):
                    MM(b[:, 0:36], hnTt[:, kc, :], rwS[:, kc, :], kc == 0, kc == 7, fast=False)
                routing(trow // 128, b)

            pd1(0)
            for tt in range(4):
                if tt + 1 < 4:
                    pd1(tt + 1)
                pd2(tt)
        top[0] = job_mark
        topR[0] = jobR_mark

    def moe_dynamic():
        zpop(1000)
        P.label = "disp"
        top[0] = pers_mark
        topR[0] = persR_mark
        cnt = A(alloc(64), 32)
        nb = A(alloc(64), 32)
        pend = A(alloc(64), 32)
        pstart = A(alloc(64), 32)
        destf = A(alloc(64), 64)
        dest_i = A(alloc(64), 64).bitcast(I32)
        bef = A(alloc(64), NB)
        be_i = A(alloc(64), NB).bitcast(I32)
        widxf = A(alloc(64), NB)
        widx = A(alloc(64), NB).bitcast(I32)
        tmp32 = [A(alloc(64), 32) for _ in range(4)]
        mark = top[0]
        rank_all = A(alloc(1024), 1024).rearrange("p (t e) -> p t e", e=32)
        cums = A(alloc(1024), 1024).rearrange("p (t e) -> p t e", e=32)
        MEMSET("dve", cums[:, 0, :], 0.0)
        for i in range(1, 32):
            TT("dve", cums[:, i, :], cums[:, i - 1, :], sel_all[:, i - 1, :], ALU.add)
        csum = A(alloc(64), 32)
        TT("dve", csum, cums[:, 31, :], sel_all[:, 31, :], ALU.add)
        bk = [bank(), bank()]
        for i in range(32):
            o = bk[i // 16][:, (i % 16) * 32:(i % 16 + 1) * 32]
            MM(o, Lst, sel_all[:, i, :], True, i == 0, fast=False)
            if i > 0:
                MM(o, ones, cums[:, i, :], False, True, fast=False)
        bc_ = bank()
        MM(bc_[:, 0:32], ones, csum, True, True, fast=False)
        CP("dve", rank_all[:, 0:16, :], bk[0].rearrange("p (t e) -> p t e", e=32))
        CP("dve", rank_all[:, 16:32, :], bk[1].rearrange("p (t e) -> p t e", e=32))
        CP("dve", cnt, bc_[:, 0:32])
        MEMSET("dve", nb, 0.0)
        for m in range((SEQ + BS - 1) // BS):
            STT(nb, cnt, float(m * BS), nb, ALU.is_gt, ALU.add)
        P.add("dve", lambda e: e.tensor_tensor_scan(out=pend, data0=ones[:, 0:32], data1=nb, initial=0.0,
                                                    op0=ALU.mult, op1=ALU.add), [ones, nb], [pend])
        TS("dve", pend, pend, float(BS), ALU.mult)
        STT(pstart, nb, -float(BS), pend, ALU.mult, ALU.add)
        for i in range(32):
            t_ = tmp32[0]
            p1 = tmp32[1]
            p2 = tmp32[2]
            d12 = sm(1)
            TT("dve", t_, rank_all[:, i, :], pstart, ALU.add)
            TT("dve", p1, oh1_all[:, i, :], t_, ALU.mult)
            TT("dve", p2, sel_all[:, i, :], t_, ALU.mult)
            d1 = destf[:, 2 * i:2 * i + 1]
            d2 = destf[:, 2 * i + 1:2 * i + 2]
            P.add("dve", (lambda d1, p1: (lambda e: e.tensor_reduce(out=d1, in_=p1, axis=AX.X, op=ALU.add)))(d1, p1), [p1], [d1])
            P.add("dve", (lambda d12, p2: (lambda e: e.tensor_reduce(out=d12, in_=p2, axis=AX.X, op=ALU.add)))(d12, p2), [p2], [d12])
            TT("dve", d2, d12, d1, ALU.subtract)
        CP("dve", dest_i, destf)
        MEMSET("dve", bef, 0.0)
        for e_ in range(32):
            STT(bef, bstart, pend[:, e_:e_ + 1], bef, ALU.is_ge, ALU.add)
        CP("dve", be_i, bef)
        STT(widxf, bef, 128.0, A(c_cst + C_PI, NB), ALU.mult, ALU.add)
        CP("dve", widx, widxf)
        if debug:
            DMA("sp", c_dbg[:, 0:64], destf)
            DMA("sp", c_dbg[:, 64:64 + NB], bef)
            DMA("sp", c_dbg[:, 128:192], w12)
            DMA("sp", c_dbg[:, 192:224], cnt)
        top[0] = mark
        tok_s = A(alloc(512), 512).bitcast(I32).rearrange("p (t c) -> p t c", c=16)
        DMA("sp", tok_s, tokid.rearrange("p (t c) -> p t c", c=16))
        DMA("sp", inv_d, zi)
        for i in range(32):
            for k in range(2):
                idx = dest_i[:, 2 * i + k:2 * i + k + 1]
                src = tok_s[:, i, :]
                P.add("pool", (lambda idx, src: (lambda e: e.indirect_dma_start(
                    out=inv_d, out_offset=bass.IndirectOffsetOnAxis(ap=idx, axis=0), in_=src, in_offset=None)))(idx, src),
                    [idx, src], [inv_d], dma=True, merge=True, after=[inv_d])
        c_ring = allocR(20480)
        ring_i = [0]

        def ring():
            i = ring_i[0]
            ring_i[0] = (i + 1) % 5
            return AR(c_ring + i * 4096, 4096)

        c_xT = allocR(8 * BS)
        xT = AR(c_xT, 8 * BS).rearrange("p (k t) -> p k t", k=8)
        hid = AR(allocR(4 * BS), 4 * BS).rearrange("p (k t) -> p k t", k=4)
        xrow = [AR(allocR(1024), 1024) for _ in range(ST)]
        ysb = [A(alloc(1024), 1024) for _ in range(4)]
        sgt = [A(alloc(BS), BS) for _ in range(2)]
        invS = [A(alloc(64), 16).bitcast(I32) for _ in range(8)]
        bcreg = {}

        def gather_w(b_, tab):
            o = ring()
            idx = widx[:, b_:b_ + 1]

            def fn(e, idx=idx, o=o, tab=tab):
                if "r" not in bcreg:
                    rg = e.alloc_register("wbound")
                    e.reg_mov(rg, 4095)
                    bcreg["r"] = rg
                return e.indirect_dma_start(out=r_(o), out_offset=None, in_=tab,
                                            in_offset=bass.IndirectOffsetOnAxis(ap=idx, axis=0),
                                            bounds_check=bcreg["r"], oob_is_err=False)

            P.add("pool", fn, [idx], [o], dma=True)
            return o

        def loads(b_):
            w2 = [gather_w(b_, ewg), gather_w(b_, ewu)]
            for st in range(ST):
                ixf = invS[(b_ * ST + st) % 8]
                DMA("sp", ixf, inv_d[b_ * BS + st * 128:b_ * BS + (st + 1) * 128, :])
                ix = ixf[:, 0:1]
                P.add("pool", (lambda ix, o: (lambda e: e.indirect_dma_start(
                    out=r_(o), out_offset=None, in_=hn_all, in_offset=bass.IndirectOffsetOnAxis(ap=ix, axis=0))))(ix, xrow[st]),
                    [ix, hn_all], [xrow[st]], dma=True)
            return w2

        P.label = "exp"
        nxt = loads(0)
        nxt_wd = gather_w(0, ewd)
        for b_ in range(NB):
            cur = nxt + [nxt_wd]
            wg3 = cur[0].rearrange("p (k n) -> p k n", k=8)
            wu3 = cur[1].rearrange("p (k n) -> p k n", k=8)
            wd3 = cur[2].rearrange("p (k n) -> p k n", k=4)
            for kc in range(8):
                bkk = bank()
                for st in range(ST):
                    TR(bkk[:, st * 128:(st + 1) * 128], xrow[st][:, kc * 128:(kc + 1) * 128], ident)
                if kc % 2 == 0:
                    CP("dve", r_(xT[:, kc, :]), bkk[:, 0:BS])
                else:
                    ACT(r_(xT[:, kc, :]), bkk[:, 0:BS], AF.Copy)
            if debug and b_ == 0:
                DMA("sp", hnT_all[0, :, :], cur[0])
                DMA("sp", hnT_all[1, :, :], cur[2])
                DMA("sp", hnT_all[2, :, 0:8 * BS], AR(c_xT, 8 * BS))
            if b_ + 1 < NB:
                nxt = loads(b_ + 1)
            for hc in range(4):
                bG = bank()
                for kc in range(8):
                    MM(bG[:, 0:BS], wg3[:, kc, hc * 128:(hc + 1) * 128], xT[:, kc, :], kc == 0, kc == 7)
                bU = bank()
                for kc in range(8):
                    MM(bU[:, 0:BS], wu3[:, kc, hc * 128:(hc + 1) * 128], xT[:, kc, :], kc == 0, kc == 7)
                sg = sgt[hc % 2]
                ACT(sg, bG[:, 0:BS], AF.Silu)
                TT("dve", r_(hid[:, hc, :]), sg, bU[:, 0:BS], ALU.mult)
            if b_ + 1 < NB:
                nxt_wd = gather_w(b_ + 1, ewd)
            for st in range(ST):
                yb_ = ysb[(b_ * ST + st) % 4]
                for fh in range(2):
                    bb = bank()
                    for hc in range(4):
                        MM(bb, hid[:, hc, st * 128:(st + 1) * 128], wd3[:, hc, fh * 512:(fh + 1) * 512], hc == 0, hc == 3)
                    if fh == 0:
                        CP("dve", yb_[:, 0:512], bb)
                    else:
                        ACT(yb_[:, 512:1024], bb, AF.Copy)
                DMA("act", ys_d[b_ * BS + st * 128:b_ * BS + (st + 1) * 128, :], yb_)
        P.label = "comb"
        top[0] = mark
        nfin = A(alloc(1024), 1024)
        DMA("sp", nfin, bc[:, B_NF:B_NF + 1024])
        yg = [A(alloc(1024), 1024) for _ in range(6)]
        hb = [A(alloc(1024), 1024) for _ in range(3)]
        sqs = {}

        def cb1(i):
            ht = hb[i % 3]
            DMA("sp", ht, h_all[i * 128:(i + 1) * 128, :])
            for k in range(2):
                idx = dest_i[:, 2 * i + k:2 * i + k + 1]
                yk = yg[(i % 3) * 2 + k]
                P.add("pool", (lambda idx, yk: (lambda e: e.indirect_dma_start(
                    out=yk, out_offset=None, in_=ys_d, in_offset=bass.IndirectOffsetOnAxis(ap=idx, axis=0))))(idx, yk),
                    [idx, ys_d], [yk], dma=True)
                STT(ht, yk, w12[:, 2 * i + k:2 * i + k + 1], ht, ALU.mult, ALU.add)
            st = sm(12)
            for j in range(2):
                sj = st[:, 6 * j:6 * j + 6]
                src = ht[:, j * 512:(j + 1) * 512]
                P.add("dve", (lambda sj, src: (lambda e: e.bn_stats(out=sj, in_=src)))(sj, src), [src], [sj])
            mv = sm(2)
            P.add("dve", (lambda mv, st: (lambda e: e.bn_aggr(out=mv, in_=st)))(mv, st), [st], [mv])
            t = sm(1)
            STT(t, mv[:, 0:1], mv[:, 0:1], mv[:, 1:2], ALU.mult, ALU.add)
            t2 = sm(1)
            TS("dve", t2, t, EPS, ALU.add)
            sq = sm(1)
            ACT(sq, t2, AF.Sqrt)
            sqs[i] = sq

        def cb2(i):
            ht = hb[i % 3]
            sq = sqs.pop(i)
            r = sm(1)
            P.add("dve", (lambda r, sq: (lambda e: e.reciprocal(out=r, in_=sq)))(r, sq), [sq], [r])
            STT(ht, ht, r, nfin, ALU.mult, ALU.mult)
            DMA("act", out[i * 128:(i + 1) * 128, :], ht)

        cb1(0)
        for i in range(32):
            if i + 1 < 32:
                cb1(i + 1)
            cb2(i)

    nq = NQ if stage >= 2 else 1
    for q in range(nq):
        cs, sn, nmark = phase_A(q)
        if stage >= 1:
            gdT, wgd3 = job_gdown(q)
            for half in range(2):
                b = bank()
                for kc in range(8):
                    MM(b[0:16, :], wgd3[:, kc, :], uT[:, kc, half * 512:(half + 1) * 512], kc == 0, kc == 7, fast=False)
                CP("dve", gdT[:, half * 512:(half + 1) * 512], b[0:16, :])
            for h in range(4):
                job_gla(q, h, gdT)
            while rot_steps:
                rot_steps.pop(0)()
            top[0] = nmark
            for h in range(4):
                job_ret(q, h, cs, sn)
        if stage >= 2:
            job_mg(q)
            post(q)
    if stage >= 3:
        moe_dynamic()
    if debug and stage < 2:
        DMA("pool", hnT_all[:, :, 0:QT].rearrange("k p t -> p k t"), uT)

    P.emit(nc, stack)
    stack.close()
    nc._prog_labels = P.labels
    return nc


def _f_layout(W):
    K, M = W.shape
    return np.ascontiguousarray(W.reshape(K // 128, 128, M // 128, 128).transpose(2, 1, 0, 3)).reshape(M // 128, 128, (K // 128) * 128)


def _t_layout(W):
    K, N = W.shape
    return np.ascontiguousarray(W.reshape(K // 128, 128, N // 512, 512).transpose(2, 1, 0, 3)).reshape(N // 512, 128, (K // 128) * 512)


def _e_layout(W):
    E, K, N = W.shape
    return np.ascontiguousarray(W.reshape(E, K // 128, 128, N).transpose(0, 2, 1, 3)).reshape(E * 128, (K // 128) * N)


def _consts():
    c = np.zeros((128, NCST), np.float64)
    c[:, C_ID:C_ID + 128] = np.eye(128)
    j = np.arange(128)[:, None]
    i = np.arange(128)[None, :]
    c[:, C_GM:C_GM + 128] = (j <= i)
    for h in range(4):
        gam = 1.0 - 2.0 ** (-5.0 - h)
        c[:, C_RM + h * 128:C_RM + (h + 1) * 128] = np.where(j <= i, gam ** (-(j + 1.0)), 0.0)
        c[:, C_RV + h] = gam ** (np.arange(128) + 1.0)
        c[:, C_RV + 4 + h] = gam ** (2.0 * (np.arange(128) + 1.0))
        c[:, C_RV + 8 + h] = gam ** (127.0 - np.arange(128))
    c[:, C_LS:C_LS + 128] = (j < i)
    c[:, C_BS:C_BS + NB] = np.arange(NB)[None, :] * float(BS)
    c[:, C_PI:C_PI + 64] = np.arange(128)[:, None]
    th = 1.0 / (np.float32(10000.0) ** np.linspace(0.0, 1.0, 128, dtype=np.float32))
    c[:, C_TH] = th.astype(np.float64)
    return c.astype(np.float32)


def prep_inputs(inputs):
    f = lambda a: np.asarray(a, dtype=np.float32)
    w_in = f(inputs["w_in"])[0]
    slabs = [None] * NF
    for h in range(4):
        slabs[F_GQ[h]] = w_in[:, O_GQ + h * 128:O_GQ + (h + 1) * 128]
        slabs[F_GK[h]] = w_in[:, O_GK + h * 128:O_GK + (h + 1) * 128]
        rq = w_in[:, O_RQ + h * 256:O_RQ + (h + 1) * 256]
        rk = w_in[:, O_RK + h * 256:O_RK + (h + 1) * 256]
        slabs[F_RQE[h]] = rq[:, 0::2]
        slabs[F_RQO[h]] = rq[:, 1::2]
        slabs[F_RKE[h]] = rk[:, 0::2]
        slabs[F_RKO[h]] = rk[:, 1::2]
    for i in range(8):
        slabs[F_MGA[i]] = w_in[:, O_MGA + i * 128:O_MGA + (i + 1) * 128]
        slabs[F_MGB[i]] = w_in[:, O_MGB + i * 128:O_MGB + (i + 1) * 128]
    wF = np.stack([_f_layout(np.ascontiguousarray(s))[0] for s in slabs])
    wgd = np.ascontiguousarray(w_in[:, O_GD:O_GD + 16].reshape(8, 128, 16).transpose(1, 0, 2)).reshape(128, 128)
    groups = [None] * NT
    for h in range(4):
        groups[T_GLA[h]] = np.concatenate([w_in[:, O_GV + h * 256:O_GV + (h + 1) * 256],
                                           w_in[:, O_GG + h * 256:O_GG + (h + 1) * 256]], axis=1)
        groups[T_RV[h]] = w_in[:, O_RV + h * 512:O_RV + (h + 1) * 512]
        groups[T_RG[h]] = w_in[:, O_RG + h * 512:O_RG + (h + 1) * 512]
    wT = np.stack([_t_layout(np.ascontiguousarray(g))[0] for g in groups])
    wbg = _f_layout(f(inputs["w_branch_gla"])[0])
    wbr = _f_layout(f(inputs["w_branch_ret"])[0])
    wo = _t_layout(f(inputs["w_out"])[0])
    rwf = np.concatenate([f(inputs["router_group_w"])[0],
                          f(inputs["router_expert_w"])[0].transpose(1, 0, 2).reshape(1024, 32)], axis=1)
    rw = np.ascontiguousarray(rwf.reshape(8, 128, 36).transpose(1, 0, 2)).reshape(128, 288)
    cst = _consts()
    cst[:, C_NMW:C_NMW + 8] = f(inputs["norm_mix_w"])[0].reshape(8, 128).T
    cst[:, C_NFW:C_NFW + 8] = f(inputs["norm_ffn_w"])[0].reshape(8, 128).T
    cst[:, C_GKB:C_GKB + 4] = f(inputs["gla_gk_bias"])[0].reshape(4, 128).T
    bcv = np.zeros((NBC,), np.float32)
    bcv[B_GNW:B_GNW + 256] = f(inputs["gla_norm_w"])[0]
    bcv[B_RNW:B_RNW + 2048] = f(inputs["ret_norm_w"])[0]
    bcv[B_NF:B_NF + 1024] = f(inputs["norm_final_w"])
    bcv[B_NFW:B_NFW + 1024] = f(inputs["norm_ffn_w"])[0]
    bcv[B_RB:B_RB + 4] = f(inputs["router_group_b"])[0]
    bcv[B_RB + 4:B_RB + 36] = f(inputs["router_expert_b"])[0].reshape(32)
    bcr = np.ascontiguousarray(np.broadcast_to(bcv[None, :], (128, NBC)))
    tok = (np.arange(32, dtype=np.int32)[None, :, None] * 128 + np.arange(128, dtype=np.int32)[:, None, None])
    tok = np.ascontiguousarray(np.broadcast_to(tok, (128, 32, 16))).reshape(128, 512)
    shared = dict(zr=np.zeros((512, D), np.float32), zi=np.zeros((NSLOT, 16), np.int32), tokid=tok, cst=cst, bc=bcr, wF=wF, wgd=wgd, wT=wT, wbg=wbg, wbr=wbr, wo=wo, rw=rw,
                  gku=f(inputs["gla_gk_up"])[0],
                  ewg=_e_layout(f(inputs["expert_w_gate"])[0]), ewu=_e_layout(f(inputs["expert_w_up"])[0]),
                  ewd=_e_layout(f(inputs["expert_w_down"])[0]))
    xs = f(inputs["x"])
    ps = np.asarray(inputs["positions"]).astype(np.int32)
    in_maps = []
    for c in range(NCORES):
        m = dict(shared)
        m["x"] = np.ascontiguousarray(xs[c])
        m["pos"] = np.ascontiguousarray(np.broadcast_to(ps[c][None, :], (128, SEQ)))
        in_maps.append(m)
    return in_maps


def kernel(**inputs):
    in_maps = prep_inputs(inputs)
    nc = build()
    res = run_bass_kernel_spmd(nc, in_maps, core_ids=list(range(NCORES)))
    return np.stack([np.asarray(r["out"], dtype=np.float32) for r in res.results], axis=0)
```
